# Optimizing a Trainium2 kernel written in Bass

```python
import math
import jax, jax.numpy as jnp
from jax import lax
import numpy as np

D_MODEL = 2048
BATCH = 2
SEQ = 16384
DEPTH = 4

GRID_W = 64
CTX_LEN = 256
N_MIXERS = 4
MIXER_CONV, MIXER_MLA, MIXER_POOL, MIXER_DIFF = 0, 1, 2, 3
EPS = 1e-6
ROPE_THETA = 10000.0
Q_BLOCK = 128
N_MOD = 6
CONV_WIDTH = 31
MLA_HEADS = D_MODEL // 128
MLA_NOPE = 128
MLA_ROPE = 64
MLA_V = 128
MLA_Q_RANK = 3 * D_MODEL // 8
MLA_KV_RANK = D_MODEL // 4
POOL_WINDOWS = (2, 4, 8, 16)
POOL_GROUP = D_MODEL // len(POOL_WINDOWS)
DIFF_HEAD_DIM = 128
DIFF_HEADS = D_MODEL // (2 * DIFF_HEAD_DIM)
N_EXPERTS = 16
EC_FACTOR = 2
EXPERT_FF = D_MODEL // 2

kernel_name = 'hybrid_diffusion_conv_mla_pool_diffattn_ecmoe'


def _rms(x, g):
    xf = x.astype(jnp.float32)
    y = xf * lax.rsqrt(jnp.mean(xf * xf, axis=-1, keepdims=True) + EPS)
    return (y * g.astype(jnp.float32)).astype(x.dtype)


def _layernorm(x, g, b):
    xf = x.astype(jnp.float32)
    mu = jnp.mean(xf, axis=-1, keepdims=True)
    var = jnp.mean(jnp.square(xf - mu), axis=-1, keepdims=True)
    y = (xf - mu) * lax.rsqrt(var + EPS)
    return (y * g.astype(jnp.float32) + b.astype(jnp.float32)).astype(x.dtype)


def _axial_rope_table(rows, rot_dim):
    quarter = rot_dim // 4
    inv = ROPE_THETA ** (-jnp.arange(quarter, dtype=jnp.float32) / quarter)
    row_ang = jnp.arange(rows, dtype=jnp.float32)[:, None, None] * inv
    col_ang = jnp.arange(GRID_W, dtype=jnp.float32)[None, :, None] * inv
    ang = jnp.concatenate([jnp.broadcast_to(row_ang, (rows, GRID_W, quarter)),
                           jnp.broadcast_to(col_ang, (rows, GRID_W, quarter))], axis=-1)
    ang = ang.reshape(rows * GRID_W, 2 * quarter)
    return jnp.cos(ang), jnp.sin(ang)


def _rope(x, cos, sin):
    half = x.shape[-1] // 2
    xf = x.astype(jnp.float32)
    x1, x2 = xf[..., :half], xf[..., half:]
    return jnp.concatenate([x1 * cos - x2 * sin, x1 * sin + x2 * cos], axis=-1).astype(x.dtype)


def _sweep_query_blocks(fn, q):
    b, s = q.shape[0], q.shape[1]
    nb = s // Q_BLOCK
    qb = jnp.moveaxis(q.reshape((b, nb, Q_BLOCK) + q.shape[2:]), 1, 0)
    out = jnp.moveaxis(lax.map(fn, qb), 0, 1)
    return out.reshape((b, s) + out.shape[3:])


def _softmax_attend(q, k, v, scale):
    s = jnp.einsum('bqhd,bkhd->bhqk', q, k).astype(jnp.float32) * scale
    p = jax.nn.softmax(s, axis=-1).astype(v.dtype)
    return jnp.einsum('bhqk,bkhd->bqhd', p, v)


def _diff_attend(q, k, v, lam, scale):
    s = jnp.einsum('bqhnd,bkhnd->nbhqk', q, k).astype(jnp.float32) * scale
    p = jax.nn.softmax(s, axis=-1)
    a = (p[0] - lam * p[1]).astype(v.dtype)
    return jnp.einsum('bhqk,bkhd->bqhd', a, v)


def _conv_module(h, pw1_w, pw1_b, dw_w, dw_b, ln_g, ln_b, pw2_w, pw2_b):
    u = h @ pw1_w + pw1_b
    u = u[..., :D_MODEL] * jax.nn.sigmoid(u[..., D_MODEL:])
    pad = CONV_WIDTH // 2
    u = lax.conv_general_dilated(u, dw_w[:, None, :], window_strides=(1,), padding=[(pad, pad)],
                                 dimension_numbers=('NWC', 'WIO', 'NWC'),
                                 feature_group_count=D_MODEL) + dw_b
    u = jax.nn.silu(_layernorm(u, ln_g, ln_b))
    return u @ pw2_w + pw2_b


def _pool_mixer(h, pool_w, pool_scale):
    b, l, _ = h.shape
    cs = jnp.pad(jnp.cumsum(h.astype(jnp.float32), axis=1), ((0, 0), (1, 0), (0, 0)))
    t = jnp.arange(l)
    parts = []
    for g, w in enumerate(POOL_WINDOWS):
        sl = cs[..., g * POOL_GROUP:(g + 1) * POOL_GROUP]
        lo = jnp.clip(t - w // 2, 0, l)
        hi = jnp.clip(t - w // 2 + w, 0, l)
        mean = (sl[:, hi] - sl[:, lo]) / (hi - lo).astype(jnp.float32)[:, None]
        parts.append(mean.astype(h.dtype) - h[..., g * POOL_GROUP:(g + 1) * POOL_GROUP])
    d = jnp.stack(parts, axis=2)
    y = jnp.einsum('blgi,gio->blgo', d, pool_w).reshape(b, l, D_MODEL)
    return y * pool_scale


def _mla_q(h, w_dq, q_norm, w_uq):
    b, l, _ = h.shape
    cq = _rms(h @ w_dq, q_norm)
    return (cq @ w_uq).reshape(b, l, MLA_HEADS, MLA_NOPE + MLA_ROPE)


def _mla_kv(h, w_dkv, kv_norm, w_ukv, cos=None, sin=None):
    b, l, _ = h.shape
    kv_a = h @ w_dkv
    ckv = _rms(kv_a[..., :MLA_KV_RANK], kv_norm)
    k_rope = kv_a[..., MLA_KV_RANK:]
    if cos is not None:
        k_rope = _rope(k_rope, cos, sin)
    kv = (ckv @ w_ukv).reshape(b, l, MLA_HEADS, MLA_NOPE + MLA_V)
    k = jnp.concatenate([kv[..., :MLA_NOPE],
                         jnp.broadcast_to(k_rope[:, :, None, :], (b, l, MLA_HEADS, MLA_ROPE))], axis=-1)
    return k, kv[..., MLA_NOPE:]


def _mla_mixer(n_l, n_c, need_ctx, cos, sin, w_dq, q_norm, w_uq, w_dkv, kv_norm, w_ukv, w_o):
    b, s, _ = n_l.shape
    scale = (MLA_NOPE + MLA_ROPE) ** -0.5
    k_l, v_l = _mla_kv(n_l, w_dkv, kv_norm, w_ukv, cos, sin)
    k_c, v_c = _mla_kv(n_c, w_dkv, kv_norm, w_ukv)
    k_all = jnp.concatenate([k_l, k_c], axis=1)
    v_all = jnp.concatenate([v_l, v_c], axis=1)
    q_l = _mla_q(n_l, w_dq, q_norm, w_uq)
    q_l = jnp.concatenate([q_l[..., :MLA_NOPE],
                           _rope(q_l[..., MLA_NOPE:], cos[:, None, :], sin[:, None, :])], axis=-1)
    o_l = _sweep_query_blocks(lambda qb: _softmax_attend(qb, k_all, v_all, scale), q_l)
    y_l = o_l.reshape(b, s, MLA_HEADS * MLA_V) @ w_o
    y_c = None
    if need_ctx:
        q_c = _mla_q(n_c, w_dq, q_norm, w_uq)
        o_c = _softmax_attend(q_c, k_c, v_c, scale)
        y_c = o_c.reshape(b, n_c.shape[1], MLA_HEADS * MLA_V) @ w_o
    return y_l, y_c


def _diff_project(h, w_qkv):
    b, l, _ = h.shape
    q, k, v = jnp.split(h @ w_qkv, 3, axis=-1)
    return (q.reshape(b, l, DIFF_HEADS, 2, DIFF_HEAD_DIM),
            k.reshape(b, l, DIFF_HEADS, 2, DIFF_HEAD_DIM),
            v.reshape(b, l, DIFF_HEADS, 2 * DIFF_HEAD_DIM))


def _diff_mixer(n_l, n_c, need_ctx, layer_idx, cos, sin, w_qkv, lq1, lk1, lq2, lk2, subln_g, w_o):
    b, s, _ = n_l.shape
    scale = DIFF_HEAD_DIM ** -0.5
    lam_init = 0.8 - 0.6 * math.exp(-0.3 * layer_idx)
    f32 = jnp.float32
    lam = (jnp.exp(jnp.sum(lq1.astype(f32) * lk1.astype(f32)))
           - jnp.exp(jnp.sum(lq2.astype(f32) * lk2.astype(f32))) + lam_init)
    q_l, k_l, v_l = _diff_project(n_l, w_qkv)
    q_l = _rope(q_l, cos[:, None, None, :], sin[:, None, None, :])
    k_l = _rope(k_l, cos[:, None, None, :], sin[:, None, None, :])
    q_c, k_c, v_c = _diff_project(n_c, w_qkv)
    k_all = jnp.concatenate([k_l, k_c], axis=1)
    v_all = jnp.concatenate([v_l, v_c], axis=1)
    o_l = _sweep_query_blocks(lambda qb: _diff_attend(qb, k_all, v_all, lam, scale), q_l)
    o_l = _rms(o_l, subln_g) * (1.0 - lam_init)
    y_l = o_l.reshape(b, s, DIFF_HEADS * 2 * DIFF_HEAD_DIM) @ w_o
    y_c = None
    if need_ctx:
        o_c = _rms(_diff_attend(q_c, k_c, v_c, lam, scale), subln_g) * (1.0 - lam_init)
        y_c = o_c.reshape(b, n_c.shape[1], DIFF_HEADS * 2 * DIFF_HEAD_DIM) @ w_o
    return y_l, y_c


def _expert_choice_ffn(h, router_w, w_gate, w_up, w_down):
    b, n, _ = h.shape
    cap = max(1, EC_FACTOR * n // N_EXPERTS)
    aff = jax.nn.softmax((h @ router_w).astype(jnp.float32), axis=-1)
    g, idx = lax.top_k(jnp.swapaxes(aff, 1, 2), cap)
    bidx = jnp.arange(b)[:, None, None]
    xs = h[bidx, idx]
    a = jnp.einsum('becd,edf->becf', xs, w_gate)
    u = jnp.einsum('becd,edf->becf', xs, w_up)
    y = jnp.einsum('becf,efd->becd', jax.nn.silu(a) * u, w_down)
    y = (y * g[..., None].astype(y.dtype)).astype(h.dtype)
    return jnp.zeros_like(h).at[bidx, idx].add(y)


def setup_inputs(seed: int = 0) -> dict:
    key = jax.random.key(seed)
    keys = jax.random.split(key, 48)
    ki = iter(range(48))

    def nrm(shape, scale):
        return jax.random.normal(keys[next(ki)], shape, jnp.float32) * scale

    def gain(shape):
        return 1.0 + nrm(shape, 0.02)

    D = D_MODEL
    return {
        'x': nrm((BATCH, SEQ, D), 1.0),
        'c': nrm((BATCH, D), 1.0),
        'ctx': nrm((BATCH, CTX_LEN, D), 1.0),
        'c_ctx': nrm((D,), 1.0),
        'ada_w': nrm((DEPTH, D, N_MOD * D), 0.5 * D ** -0.5),
        'ada_b': nrm((DEPTH, N_MOD * D), 0.02),
        'norm_g': gain((DEPTH, 2, D)),
        'final_g': gain((D,)),
        'conv_pw1_w': nrm((D, 2 * D), D ** -0.5),
        'conv_pw1_b': nrm((2 * D,), 0.02),
        'conv_dw_w': nrm((CONV_WIDTH, D), CONV_WIDTH ** -0.5),
        'conv_dw_b': nrm((D,), 0.02),
        'conv_ln_g': gain((D,)),
        'conv_ln_b': nrm((D,), 0.02),
        'conv_pw2_w': nrm((D, D), D ** -0.5),
        'conv_pw2_b': nrm((D,), 0.02),
        'mla_w_dq': nrm((D, MLA_Q_RANK), D ** -0.5),
        'mla_q_norm': gain((MLA_Q_RANK,)),
        'mla_w_uq': nrm((MLA_Q_RANK, MLA_HEADS * (MLA_NOPE + MLA_ROPE)), MLA_Q_RANK ** -0.5),
        'mla_w_dkv': nrm((D, MLA_KV_RANK + MLA_ROPE), D ** -0.5),
        'mla_kv_norm': gain((MLA_KV_RANK,)),
        'mla_w_ukv': nrm((MLA_KV_RANK, MLA_HEADS * (MLA_NOPE + MLA_V)), MLA_KV_RANK ** -0.5),
        'mla_w_o': nrm((MLA_HEADS * MLA_V, D), (MLA_HEADS * MLA_V) ** -0.5),
        'pool_w': nrm((len(POOL_WINDOWS), POOL_GROUP, POOL_GROUP), POOL_GROUP ** -0.5),
        'pool_scale': gain((D,)),
        'diff_w_qkv': nrm((D, 3 * DIFF_HEADS * 2 * DIFF_HEAD_DIM), D ** -0.5),
        'diff_lq1': nrm((DIFF_HEAD_DIM,), 0.1),
        'diff_lk1': nrm((DIFF_HEAD_DIM,), 0.1),
        'diff_lq2': nrm((DIFF_HEAD_DIM,), 0.1),
        'diff_lk2': nrm((DIFF_HEAD_DIM,), 0.1),
        'diff_subln_g': gain((2 * DIFF_HEAD_DIM,)),
        'diff_w_o': nrm((DIFF_HEADS * 2 * DIFF_HEAD_DIM, D), (DIFF_HEADS * 2 * DIFF_HEAD_DIM) ** -0.5),
        'moe_router': nrm((DEPTH, D, N_EXPERTS), D ** -0.5),
        'moe_w_gate': nrm((DEPTH, N_EXPERTS, D, EXPERT_FF), D ** -0.5),
        'moe_w_up': nrm((DEPTH, N_EXPERTS, D, EXPERT_FF), D ** -0.5),
        'moe_w_down': nrm((DEPTH, N_EXPERTS, EXPERT_FF, D), EXPERT_FF ** -0.5),
    }


def reference(x, c, ctx, c_ctx, ada_w, ada_b, norm_g, final_g,
              conv_pw1_w, conv_pw1_b, conv_dw_w, conv_dw_b, conv_ln_g, conv_ln_b, conv_pw2_w, conv_pw2_b,
              mla_w_dq, mla_q_norm, mla_w_uq, mla_w_dkv, mla_kv_norm, mla_w_ukv, mla_w_o,
              pool_w, pool_scale,
              diff_w_qkv, diff_lq1, diff_lk1, diff_lq2, diff_lk2, diff_subln_g, diff_w_o,
              moe_router, moe_w_gate, moe_w_up, moe_w_down):
    b, s, _ = x.shape
    rows = s // GRID_W
    cos_mla, sin_mla = _axial_rope_table(rows, MLA_ROPE)
    cos_diff, sin_diff = _axial_rope_table(rows, DIFF_HEAD_DIM)
    silu_c = jax.nn.silu(c)
    silu_cc = jax.nn.silu(c_ctx)
    x_lat, x_ctx = x, ctx
    for i in range(DEPTH):
        kind = i % N_MIXERS
        need_ctx_out = i < DEPTH - 1
        ctx_needed = need_ctx_out or kind in (MIXER_MLA, MIXER_DIFF)
        mod_l = jnp.split((silu_c @ ada_w[i] + ada_b[i])[:, None, :], N_MOD, axis=-1)
        n_l = _rms(x_lat, norm_g[i, 0]) * (1 + mod_l[1]) + mod_l[0]
        n_c, mod_c = None, None
        if ctx_needed:
            mod_c = jnp.split((silu_cc @ ada_w[i] + ada_b[i])[None, None, :], N_MOD, axis=-1)
            n_c = _rms(x_ctx, norm_g[i, 0]) * (1 + mod_c[1]) + mod_c[0]
        y_c = None
        if kind == MIXER_CONV:
            conv_args = (conv_pw1_w, conv_pw1_b, conv_dw_w, conv_dw_b, conv_ln_g, conv_ln_b, conv_pw2_w, conv_pw2_b)
            y_l = _conv_module(n_l, *conv_args)
            if need_ctx_out:
                y_c = _conv_module(n_c, *conv_args)
        elif kind == MIXER_MLA:
            y_l, y_c = _mla_mixer(n_l, n_c, need_ctx_out, cos_mla, sin_mla, mla_w_dq, mla_q_norm, mla_w_uq,
                                  mla_w_dkv, mla_kv_norm, mla_w_ukv, mla_w_o)
        elif kind == MIXER_POOL:
            y_l = _pool_mixer(n_l, pool_w, pool_scale)
            if need_ctx_out:
                y_c = _pool_mixer(n_c, pool_w, pool_scale)
        else:
            y_l, y_c = _diff_mixer(n_l, n_c, need_ctx_out, i, cos_diff, sin_diff, diff_w_qkv,
                                   diff_lq1, diff_lk1, diff_lq2, diff_lk2, diff_subln_g, diff_w_o)
        x_lat = x_lat + mod_l[2] * y_l
        m_l = _rms(x_lat, norm_g[i, 1]) * (1 + mod_l[4]) + mod_l[3]
        x_lat = x_lat + mod_l[5] * _expert_choice_ffn(m_l, moe_router[i], moe_w_gate[i], moe_w_up[i], moe_w_down[i])
        if need_ctx_out:
            x_ctx = x_ctx + mod_c[2] * y_c
            m_c = _rms(x_ctx, norm_g[i, 1]) * (1 + mod_c[4]) + mod_c[3]
            x_ctx = x_ctx + mod_c[5] * _expert_choice_ffn(m_c, moe_router[i], moe_w_gate[i], moe_w_up[i], moe_w_down[i])
    return _rms(x_lat, final_g)
```

```python
import math
from contextlib import ExitStack
import numpy as np
import concourse.bass as bass
import concourse.mybir as mybir
from concourse.bass_utils import run_bass_kernel_spmd

F32 = mybir.dt.float32
BF16 = mybir.dt.bfloat16
U32 = mybir.dt.uint32
I32 = mybir.dt.int32
AF = mybir.ActivationFunctionType
ALU = mybir.AluOpType
EPS = 1e-6


class Cfg:
    def __init__(self, D=2048, S=16384, CTX=256, E=16, L=4, GRID_W=64, B=2):
        self.D, self.S, self.CTX, self.E, self.L, self.GRID_W, self.B = D, S, CTX, E, L, GRID_W, B
        self.NT = S + CTX
        self.DC = D // 128
        self.FF = D // 2
        self.FC = self.FF // 128
        self.H = D // 128
        self.QR = 3 * D // 8
        self.QC = self.QR // 128
        self.KVR = D // 4
        self.KVC = self.KVR // 128
        self.HD = D // 256
        self.PG = D // 4
        self.PGC = self.PG // 128
        self.CAP = max(1, 2 * S // E)
        self.CAPC = max(1, 2 * CTX // E)
        self.CONVW = 31

    def tiles(self, ctx=True):
        out = [(t0, 512, 0) for t0 in range(0, self.S, 512)]
        if ctx:
            for t0 in range(0, self.CTX, 512):
                out.append((self.S + t0, min(512, self.CTX - t0), 1))
        return out


class Prog:
    ENGS = ("pe", "act", "dve", "pool", "sp")
    NDSEM = 8

    def __init__(self, nc):
        self.nc = nc
        self.es = ExitStack()
        self.esem = {e: self.es.enter_context(nc.semaphore("s_" + e)) for e in self.ENGS}
        self.ecnt = {e: 0 for e in self.ENGS}
        self.dsem = {q: [self.es.enter_context(nc.semaphore("d_%s%d" % (q, i))) for i in range(self.NDSEM)]
                     for q in ("sp", "pool", "act")}
        self.dcnt = {q: [0] * self.NDSEM for q in self.dsem}
        self.drr = {q: 0 for q in self.dsem}
        self.waited = {e: {} for e in self.ENGS}
        self.ops = []
        self.state = {}
        self.nphase = 0

    def _rec(self, eng, fn, r, w, dma):
        deps = set()
        for k in r:
            st = self.state.get(k)
            if st and st[0] is not None:
                deps.add(st[0])
        for k in w:
            st = self.state.get(k)
            if st:
                if st[0] is not None:
                    deps.add(st[0])
                deps.update(st[1])
        oid = len(self.ops)
        self.ops.append(dict(eng=eng, fn=fn, deps=deps, dma=dma, ms=False))
        for k in r:
            st = self.state.setdefault(k, [None, []])
            st[1].append(oid)
        for k in w:
            self.state[k] = [oid, []]
        return oid

    def op(self, eng, fn, r=(), w=()):
        return self._rec(eng, fn, r, w, False)

    def dma(self, q, out, in_, r=(), w=(), **kw):
        return self._rec(q, lambda e: e.dma_start(out=out, in_=in_, **kw), r, w, True)

    def dma_fn(self, q, fn, r=(), w=()):
        return self._rec(q, fn, r, w, True)

    def flush(self):
        nc = self.nc
        ops = self.ops
        if not ops:
            return
        for o in ops:
            for d in o["deps"]:
                dd = ops[d]
                if dd["eng"] == "pe" and o["eng"] == "pe" and not dd["dma"] and not o["dma"]:
                    continue
                dd["ms"] = True
        last = {}
        for i, o in enumerate(ops):
            last[(o["eng"], o["dma"])] = i
        for (e, isd), i in last.items():
            if not isd:
                ops[i]["ms"] = True
        for o in ops:
            e = o["eng"]
            if o["dma"]:
                i = self.drr[e]
                self.drr[e] = (i + 1) % self.NDSEM
                o["sem"] = self.dsem[e][i]
                o["prev"] = self.dcnt[e][i]
                self.dcnt[e][i] += 16
                o["val"] = self.dcnt[e][i]
            elif o["ms"]:
                self.ecnt[e] += 1
                o["sem"] = self.esem[e]
                o["val"] = self.ecnt[e]
        per = {e: [] for e in self.ENGS}
        for o in ops:
            per[o["eng"]].append(o)
        targets = []
        for e in self.ENGS:
            if self.ecnt[e] > 0:
                targets.append((self.esem[e], self.ecnt[e]))
        for q in self.dsem:
            for i in range(self.NDSEM):
                if self.dcnt[q][i] > 0:
                    targets.append((self.dsem[q][i], self.dcnt[q][i]))
        waited = self.waited

        def emit_eng(ename):
            def run(eng):
                wd = waited[ename]

                def wait(sem, val):
                    key = sem.num if hasattr(sem, "num") else id(sem)
                    if wd.get(key, 0) < val:
                        eng.wait_ge(sem, val)
                        wd[key] = val

                for o in per[ename]:
                    if o["dma"] and o["prev"] > 0:
                        wait(o["sem"], o["prev"])
                    for d in sorted(o["deps"]):
                        dd = ops[d]
                        if dd["eng"] == "pe" and ename == "pe" and not dd["dma"] and not o["dma"]:
                            continue
                        wait(dd["sem"], dd["val"])
                    ins = o["fn"](eng)
                    if o["dma"]:
                        ins.then_inc(o["sem"], 16)
                    elif o["ms"]:
                        ins.then_inc(o["sem"], 1)
                for sem, val in targets:
                    wait(sem, val)
            return run

        with nc.Block() as block:
            block.sync(emit_eng("sp"))
            block.tensor(emit_eng("pe"))
            block.scalar(emit_eng("act"))
            block.vector(emit_eng("dve"))
            block.gpsimd(emit_eng("pool"))
        self.ops = []
        self.state = {}
        self.nphase += 1


def bcast_rows(ap_row, nparts):
    t = ap_row.tensor
    return bass.AP(t, ap_row.offset, [[0, nparts]] + [list(x) for x in ap_row.ap[-1:]])


class Builder:
    def __init__(self, cfg, debug=()):
        self.cfg = cfg
        self.debug = set(debug)
        self.nc = bass.Bass("TRN2", target_bir_lowering=False)
        self.P = Prog(self.nc)
        self.inp = {}
        self.scr = {}
        self.dbg_router = True
        self.dbg_tr = True

    def din(self, name, shape, dt=F32):
        t = self.nc.dram_tensor(name, list(shape), dt, kind="ExternalInput")
        self.inp[name] = t
        return t

    def dscr(self, name, shape, dt=F32):
        kind = "ExternalOutput" if name in self.debug else "Internal"
        t = self.nc.dram_tensor(name, list(shape), dt, kind=kind)
        self.scr[name] = t
        return t

    def declare(self):
        c = self.cfg
        D, NT, L, DC, E = c.D, c.NT, c.L, c.DC, c.E
        i = self.din
        i("xT_in", [D, NT])
        i("svec", [128, DC, 2])
        i("ada_w", [L, D, 6 * D])
        i("ada_bT", [128, L, 6 * DC])
        i("normgT", [128, L, 2, DC])
        i("finalgT", [128, DC])
        i("conv_pw1_w", [D, 2 * D]); i("pw1_bT", [128, 2 * DC]); i("dw_wT", [128, DC, c.CONVW])
        i("dw_bT", [128, DC]); i("ln_gT", [128, DC]); i("ln_bT", [128, DC])
        i("conv_pw2_w", [D, D]); i("pw2_bT", [128, DC])
        i("mla_w_dq", [D, c.QR]); i("q_normT", [128, c.QC]); i("mla_w_uq", [c.QR, c.H * 192])
        i("mla_w_dkv", [D, c.KVR + 64]); i("kv_normT", [128, c.KVC]); i("mla_w_ukv", [c.KVR, c.H * 256])
        i("mla_w_o", [D, D])
        i("pool_w", [4, c.PG, c.PG]); i("pool_scaleT", [128, DC]); i("pool_inv", [4, NT])
        i("diff_w_qkv", [D, 3 * D]); i("diff_lam", [1, 4 * 128]); i("subln_gT", [128, 2]); i("diff_w_o", [D, D])
        i("rope_mla", [2, 64, NT]); i("rope_diff", [2, 128, NT])
        i("moe_router", [L, D, E]); i("moe_w_gate", [L, E, D, c.FF]); i("moe_w_up", [L, E, D, c.FF])
        i("moe_w_down", [L, E, c.FF, D])
        self.outT = self.nc.dram_tensor("outT", [D, c.S], F32, kind="ExternalOutput")
        s = self.dscr
        s("xT", [D, NT])
        s("nT", [D, NT], BF16)
        s("macc", [NT + 128, D])
        s("mtok", [NT + 128, D], BF16)
        s("affT", [E, NT])
        s("uT", [D, NT])
        HH = max(c.H, 2 * c.HD)
        s("qnT", [128, HH, NT], BF16); s("qrT", [64, c.H, NT], BF16); s("knT", [128, HH, NT], BF16)
        s("krT", [64, NT], BF16); s("vtok", [NT, D], BF16)
        RT_ = (c.CAP + c.CAPC + 127) // 128
        s("dbg_idx", [128, RT_, E], I32)
        s("dbg_g", [128, RT_, E])
        i("ident", [128, 128])
        i("padidx", [E, 128])

    def phase(self):
        b = self

        class _Ph:
            def __enter__(s2):
                b._es = ExitStack()
                b._es.__enter__()
                b._rot = {}
                return s2

            def __exit__(s2, *a):
                if a[0] is None:
                    b.P.flush()
                b._es.__exit__(*a)
                return False
        return _Ph()

    def sb(self, name, shape, dt=F32):
        return self._es.enter_context(self.nc.sbuf_tensor("%s_p%d" % (name, self.P.nphase), list(shape), dt))

    def ps(self, name, shape, dt=F32):
        return self._es.enter_context(self.nc.psum_tensor("%s_p%d" % (name, self.P.nphase), list(shape), dt))

    def sbn(self, name, n, shape, dt=F32):
        return [self.sb("%s%d" % (name, i), shape, dt) for i in range(n)]

    def psn(self, name, n, shape, dt=F32):
        return [self.ps("%s%d" % (name, i), shape, dt) for i in range(n)]

    def rot(self, name, n):
        i = self._rot.get(name, 0)
        self._rot[name] = i + 1
        return i % n

    def phase_consts(self):
        c, P, nc = self.cfg, self.P, self.nc
        L, DC = c.L, c.DC
        NJ = 6 * DC
        A = nc.alloc_sbuf_tensor
        self.ones_f = A("ones_f", [128, 128], F32)
        self.ones_b = A("ones_b", [128, 128], BF16)
        self.id_f = A("id_f", [128, 128], F32)
        self.id_b = A("id_b", [128, 128], BF16)
        self.modv = A("modv", [128, L, 2, NJ], F32)
        self.modA = A("modA", [128, L, 2, 2, DC], F32)
        self.fing = A("fing", [128, DC], F32)
        self.zero_c = A("zero_c", [128, 1], F32)
        self.eps_c = A("eps_c", [128, 1], F32)
        RT_ = (c.CAP + c.CAPC + 127) // 128
        self.idxT = A("idxT", [128, RT_, c.E], I32)
        self.gT = A("gT", [128, RT_, c.E], F32)
        with self.phase():
            s_sb = self.sb("s_sb", [128, DC, 2])
            ab = self.sb("ab", [128, L, NJ])
            ng = self.sb("ng", [128, L, 2, DC])
            W = self.sbn("adaW", 2, [128, DC, 512])
            pm = self.ps("pm", [128, NJ, 2])
            I = self.inp
            P.op("dve", lambda e: e.memset(self.ones_f[:], 1.0), w=["ones_f"])
            P.op("dve", lambda e: e.memset(self.ones_b[:], 1.0), w=["ones_b"])
            P.op("dve", lambda e: e.memset(self.zero_c[:], 0.0), w=["zero_c"])
            P.op("dve", lambda e: e.memset(self.eps_c[:], EPS), w=["eps_c"])
            P.op("dve", lambda e: e.memset(self.idxT[:], 0), w=["idxT"])
            P.op("dve", lambda e: e.memset(self.gT[:], 0.0), w=["gT"])
            P.dma("sp", self.id_f[:], I["ident"].ap(), w=["id_f"])
            P.op("dve", lambda e: e.tensor_copy(out=self.id_b[:], in_=self.id_f[:]), r=["id_f"], w=["id_b"])
            P.dma("sp", self.fing[:], I["finalgT"].ap(), w=["fing"])
            P.dma("sp", s_sb[:], I["svec"].ap(), w=["s_sb"])
            P.dma("sp", ab[:], I["ada_bT"].ap(), w=["ab"])
            P.dma("sp", ng[:], I["normgT"].ap(), w=["ng"])
            P.op("act", lambda e: e.activation(out=s_sb[:], in_=s_sb[:], func=AF.Silu), r=["s_sb"], w=["s_sb"])
            npc = (6 * c.D) // 512
            for l in range(L):
                wv = I["ada_w"].ap()[l].rearrange("(c p) n -> p c n", p=128)
                for pc in range(npc):
                    bi = self.rot("adaW", 2)
                    wk = "adaW%d" % bi
                    P.dma("sp", W[bi][:], wv[:, :, pc * 512:(pc + 1) * 512], w=[wk])
                    for j4 in range(4):
                        j = pc * 4 + j4
                        for cc in range(DC):
                            P.op("pe", lambda e, bi=bi, j=j, j4=j4, cc=cc: e.matmul(
                                pm[:, j, :], W[bi][:, cc, j4 * 128:(j4 + 1) * 128], s_sb[:, cc, :],
                                start=(cc == 0), stop=(cc == DC - 1)), r=[wk, "s_sb"], w=["pm"])
                for v in range(2):
                    P.op("dve", lambda e, l=l, v=v: e.tensor_tensor(
                        out=self.modv[:, l, v, :], in0=pm[:, :, v], in1=ab[:, l, :], op=ALU.add),
                        r=["pm", "ab"], w=["modv"])
                    for k in range(2):
                        j0 = (1 + 3 * k) * DC
                        P.op("dve", lambda e, l=l, v=v, k=k, j0=j0: e.scalar_tensor_tensor(
                            out=self.modA[:, l, v, k, :], in0=self.modv[:, l, v, j0:j0 + DC], scalar=1.0,
                            in1=ng[:, l, k, :], op0=ALU.add, op1=ALU.mult), r=["modv", "ng"], w=["modA"])

    def rsqrt(self, out, in_, scale, r, w, eps=EPS):
        P = self.P
        P.op("act", lambda e: e.activation(out=out, in_=in_, func=AF.Sqrt, scale=scale, bias=self.eps_c[:, 0:1]),
             r=list(r) + ["eps_c"], w=w)
        P.op("dve", lambda e: e.reciprocal(out=out, in_=out), r=w, w=w)

    def modcol(self, l, v, j, cc):
        return self.modv[:, l, v, j * self.cfg.DC + cc: j * self.cfg.DC + cc + 1]

    def phase_copy_in(self):
        c, P = self.cfg, self.P
        with self.phase():
            buf = self.sbn("cpy", 2, [128, c.DC, 512])
            src = self.inp["xT_in"].ap().rearrange("(c p) n -> p c n", p=128)
            dst = self.scr["xT"].ap().rearrange("(c p) n -> p c n", p=128)
            for (t0, tw, v) in c.tiles():
                bi = self.rot("cpy", 2)
                k = "cpy%d" % bi
                P.dma("sp", buf[bi][:, :, :tw], src[:, :, t0:t0 + tw], w=[k])
                P.dma("act", dst[:, :, t0:t0 + tw], buf[bi][:, :, :tw], r=[k], w=["xT"])

    def phase_norm(self, l, k, final=False, src="xT"):
        c, P = self.cfg, self.P
        DC, D = c.DC, c.D
        with self.phase():
            xin = self.sbn("xin", 2, [128, DC, 512])
            sq = self.sb("sq", [128, DC, 512])
            tmp = sq
            rs = self.sbn("rs", 2, [128, 512])
            nb = self.sbn("nb", 2, [128, DC, 512], F32 if final else BF16)
            pp = self.psn("pn", 2, [128, 512])
            xv = self.scr[src].ap().rearrange("(c p) n -> p c n", p=128)
            if final:
                ov = self.outT.ap().rearrange("(c p) n -> p c n", p=128)
            else:
                ov = self.scr["nT"].ap().rearrange("(c p) n -> p c n", p=128)
            for (t0, tw, v) in c.tiles(ctx=not final):
                bi = self.rot("xin", 2)
                xk, rk, nk, pk = "xin%d" % bi, "rs%d" % bi, "nb%d" % bi, "pn%d" % bi
                P.dma("act" if final else "sp", xin[bi][:, :, :tw], xv[:, :, t0:t0 + tw], r=[src], w=[xk])
                P.op("act", lambda e, bi=bi, tw=tw: e.activation(out=sq[:, :, :tw], in_=xin[bi][:, :, :tw],
                                                                func=AF.Square), r=[xk], w=["sq"] + ["tmp%d" % q for q in range(DC)])
                for cc in range(DC):
                    P.op("pe", lambda e, bi=bi, tw=tw, cc=cc: e.matmul(
                        pp[bi][:, :tw], self.ones_f[:, :], sq[:, cc, :tw], start=(cc == 0), stop=(cc == DC - 1)),
                        r=["sq", "ones_f"], w=[pk])
                self.rsqrt(rs[bi][:, :tw], pp[bi][:, :tw], 1.0 / D, r=[pk], w=[rk])
                for cc in range(DC):
                    P.op("dve", lambda e, bi=bi, tw=tw, cc=cc: e.tensor_tensor(
                        out=tmp[:, cc, :tw], in0=xin[bi][:, cc, :tw], in1=rs[bi][:, :tw], op=ALU.mult),
                        r=[xk, rk, "sq"], w=["tmp%d" % cc])
                    if final:
                        P.op("act", lambda e, bi=bi, tw=tw, cc=cc: e.activation(
                            out=nb[bi][:, cc, :tw], in_=tmp[:, cc, :tw], func=AF.Identity,
                            scale=self.fing[:, cc:cc + 1], bias=self.zero_c[:, 0:1]),
                            r=["tmp%d" % cc, "fing"], w=[nk + "_%d" % cc])
                    else:
                        P.op("act", lambda e, bi=bi, tw=tw, cc=cc, v=v: e.activation(
                            out=nb[bi][:, cc, :tw], in_=tmp[:, cc, :tw], func=AF.Identity,
                            scale=self.modA[:, l, v, k, cc:cc + 1], bias=self.modcol(l, v, 3 * k, cc)),
                            r=["tmp%d" % cc, "modA", "modv"], w=[nk + "_%d" % cc])
                P.dma("act", ov[:, :, t0:t0 + tw], nb[bi][:, :, :tw],
                      r=[nk + "_%d" % cc for cc in range(DC)], w=["nT"])

    def phase_norm2_router(self, l):
        c, P = self.cfg, self.P
        DC, D, E = c.DC, c.D, c.E
        with self.phase():
            xin = self.sbn("xin", 2, [128, DC, 512])
            sq = self.sb("sq", [128, DC, 512])
            tmp = sq
            rs = self.sbn("rs", 2, [128, 512])
            mf = self.sb("mf", [128, DC, 512])
            mb = self.sb("mb", [128, DC, 512], BF16)
            rw = self.sb("rw", [128, DC, E])
            ex = self.sb("ex", [E, 512])
            af = self.sbn("af", 2, [E, 512])
            pp = self.psn("pn", 2, [128, 512])
            pr = self.ps("pr", [128, 512])
            p2 = self.ps("p2", [128, 512])
            xv = self.scr["xT"].ap().rearrange("(c p) n -> p c n", p=128)
            P.dma("sp", rw[:], self.inp["moe_router"].ap()[l].rearrange("(c p) e -> p c e", p=128), w=["rw"])
            for (t0, tw, v) in c.tiles():
                bi = self.rot("xin", 2)
                xk, rk, pk = "xin%d" % bi, "rs%d" % bi, "pn%d" % bi
                P.dma("sp", xin[bi][:, :, :tw], xv[:, :, t0:t0 + tw], r=["xT"], w=[xk])
                P.op("act", lambda e, bi=bi, tw=tw: e.activation(out=sq[:, :, :tw], in_=xin[bi][:, :, :tw],
                                                                func=AF.Square), r=[xk], w=["sq"] + ["tmp%d" % q for q in range(DC)])
                for cc in range(DC):
                    P.op("pe", lambda e, bi=bi, tw=tw, cc=cc: e.matmul(
                        pp[bi][:, :tw], self.ones_f[:, :], sq[:, cc, :tw], start=(cc == 0), stop=(cc == DC - 1)),
                        r=["sq", "ones_f"], w=[pk])
                self.rsqrt(rs[bi][:, :tw], pp[bi][:, :tw], 1.0 / D, r=[pk], w=[rk])
                for cc in range(DC):
                    P.op("dve", lambda e, bi=bi, tw=tw, cc=cc: e.tensor_tensor(
                        out=tmp[:, cc, :tw], in0=xin[bi][:, cc, :tw], in1=rs[bi][:, :tw], op=ALU.mult),
                        r=[xk, rk, "sq"], w=["tmp%d" % cc])
                    P.op("act", lambda e, tw=tw, cc=cc, v=v: e.activation(
                        out=mf[:, cc, :tw], in_=tmp[:, cc, :tw], func=AF.Identity,
                        scale=self.modA[:, l, v, 1, cc:cc + 1], bias=self.modcol(l, v, 3, cc)),
                        r=["tmp%d" % cc, "modA", "modv"], w=["mf%d" % cc])
                    P.op("pool", lambda e, tw=tw, cc=cc: e.tensor_copy(out=mb[:, cc, :tw], in_=mf[:, cc, :tw]),
                         r=["mf%d" % cc], w=["mb%d" % cc])
                for cc in range(DC if self.dbg_router else 0):
                    P.op("pe", lambda e, tw=tw, cc=cc: e.matmul(
                        pr[0:E, :tw], rw[:, cc, :], mf[:, cc, :tw], start=(cc == 0), stop=(cc == DC - 1)),
                        r=["rw", "mf%d" % cc], w=["pr"])
                if self.dbg_router:
                    P.op("act", lambda e, tw=tw: e.activation(out=ex[:, :tw], in_=pr[0:E, :tw], func=AF.Exp),
                         r=["pr"], w=["ex"])
                    P.op("pe", lambda e, tw=tw: e.matmul(p2[0:E, :tw], self.ones_f[0:E, 0:E], ex[:, :tw],
                                                         start=True, stop=True), r=["ex", "ones_f"], w=["p2"])
                    ai = self.rot("af", 2)
                    ak = "af%d" % ai
                    P.op("dve", lambda e, tw=tw, ai=ai: e.reciprocal(out=af[ai][:, :tw], in_=p2[0:E, :tw]),
                         r=["p2"], w=[ak])
                    P.op("dve", lambda e, tw=tw, ai=ai: e.tensor_tensor(out=af[ai][:, :tw], in0=ex[:, :tw],
                                                                       in1=af[ai][:, :tw], op=ALU.mult),
                         r=["ex", ak], w=[ak])
                    P.dma("act", self.scr["affT"].ap()[:, t0:t0 + tw], af[ai][:, :tw], r=[ak], w=["affT"])
                P.dma("sp", self.scr["nT"].ap().rearrange("(c p) n -> p c n", p=128)[:, :, t0:t0 + tw], mb[:, :, :tw],
                      r=["mb%d" % cc for cc in range(DC)], w=["nT"])

    def phase_tok(self):
        c, P = self.cfg, self.P
        DC, D = c.DC, c.D
        with self.phase():
            mb = self.sbn("mbt", 2, [128, DC, 512], BF16)
            mt = self.sbn("mt", 2, [128, D], BF16)
            nbk = max(1, D // 1024)
            pt = [self.psn("pt%d" % i, nbk, [128, 1024], BF16) for i in range(2)]
            nv = self.scr["nT"].ap().rearrange("(c p) n -> p c n", p=128)
            zb = self.sb("zb", [128, D], BF16)
            P.op("dve", lambda e: e.memset(zb[:], 0.0), w=["zb"])
            P.dma("sp", self.scr["mtok"].ap()[c.NT:c.NT + 128, :], zb[:], r=["zb"], w=["mtok_pad"])
            for (t0, tw, v) in c.tiles():
                bi = self.rot("mbt", 2)
                bk_ = "mbt%d" % bi
                P.dma("sp", mb[bi][:, :, :tw], nv[:, :, t0:t0 + tw], r=["nT"], w=[bk_])
                for ts in range(tw // 128):
                    mi = self.rot("mt", 2)
                    mk = "mt%d" % mi
                    for cc in range(DC):
                        bk, off = divmod(cc * 128, 1024)
                        P.op("pe", lambda e, mi=mi, cc=cc, ts=ts, bk=bk, off=off, bi=bi: e.transpose(
                            pt[mi][bk][:, off:off + 128], mb[bi][:, cc, ts * 128:(ts + 1) * 128], self.id_b[:, :]),
                            r=[bk_, "id_b"], w=["pt%d_%d" % (mi, bk)])
                    for bk in range(nbk):
                        wd = min(1024, D)
                        if bk % 2 == 0:
                            P.op("act", lambda e, mi=mi, bk=bk, wd=wd: e.activation(
                                out=mt[mi][:, bk * 1024:bk * 1024 + wd], in_=pt[mi][bk][:, :wd], func=AF.Copy),
                                r=["pt%d_%d" % (mi, bk)], w=[mk + "_%d" % bk])
                        else:
                            P.op("dve", lambda e, mi=mi, bk=bk, wd=wd: e.tensor_copy(
                                out=mt[mi][:, bk * 1024:bk * 1024 + wd], in_=pt[mi][bk][:, :wd]),
                                r=["pt%d_%d" % (mi, bk)], w=[mk + "_%d" % bk])
                    r0 = t0 + ts * 128
                    P.dma("act", self.scr["mtok"].ap()[r0:r0 + 128, :], mt[mi][:, :],
                          r=[mk + "_%d" % bk for bk in range(nbk)], w=["mtok"])

    def phase_topk(self, l):
        c, P = self.cfg, self.P
        E, S, CTX, CAP, CAPC = c.E, c.S, c.CTX, c.CAP, c.CAPC
        R = CAP + CAPC
        RT = (R + 127) // 128
        RP = RT * 128
        with self.phase():
            wl = self.sb("wl", [E, S])
            wc = self.sb("wc", [E, CTX])
            mx = self.sb("mx", [E, RP])
            ix = self.sb("ix", [E, RP], U32)
            ixf = self.sb("ixf", [E, RP])
            ptpa = self.ps("ptpa", [128, 512])
            ptpb = self.ps("ptpb", [128, 512])
            P.op("dve", lambda e: e.memset(mx[:], 0.0), w=["mx"])
            P.op("dve", lambda e: e.memset(ix[:], 0), w=["ix"])
            P.dma("sp", wl[:], self.scr["affT"].ap()[:, 0:S], r=["affT"], w=["wl"])
            P.dma("sp", wc[:], self.scr["affT"].ap()[:, S:S + CTX], r=["affT"], w=["wc"])
            for (wk, work, n, base) in (("wl", wl, CAP, 0), ("wc", wc, CAPC, CAP)):
                for r8 in range(n // 8):
                    o = base + r8 * 8
                    P.op("dve", lambda e, work=work, o=o: e.max(out=mx[:, o:o + 8], in_=work[:]), r=[wk], w=["mx"])
                    P.op("dve", lambda e, work=work, o=o: e.max_index(ix[:, o:o + 8], mx[:, o:o + 8], work[:]),
                         r=[wk, "mx"], w=["ix"])
                    P.op("dve", lambda e, work=work, o=o: e.match_replace(
                        out=work[:], in_to_replace=mx[:, o:o + 8], in_values=work[:], imm_value=-1.0),
                        r=["mx"], w=[wk])
            P.op("dve", lambda e: e.tensor_copy(out=ixf[:], in_=ix[:]), r=["ix"], w=["ixf"])
            P.op("dve", lambda e: e.tensor_scalar(out=ixf[:, CAP:R], in0=ixf[:, CAP:R], scalar1=float(S), scalar2=None,
                                                  op0=ALU.add), r=["ixf"], w=["ixf"])
            if RP > R:
                P.dma("sp", ixf[:, R:RP], self.inp["padidx"].ap()[:, R - (RT - 1) * 128:128], r=["ixf"], w=["ixf"])
            for rt in range(RT):
                P.op("pe", lambda e, rt=rt: e.transpose(
                    ptpa[:, 0:E], ixf[:, rt * 128:(rt + 1) * 128], self.id_f[0:E, 0:E]),
                    r=["ixf", "id_f"], w=["ptpa"])
                P.op("pe", lambda e, rt=rt: e.transpose(
                    ptpb[:, 0:E], mx[:, rt * 128:(rt + 1) * 128], self.id_f[0:E, 0:E]),
                    r=["mx", "id_f"], w=["ptpb"])
                P.op("dve", lambda e, rt=rt: e.tensor_copy(out=self.idxT[:, rt, :], in_=ptpa[:, 0:E]),
                     r=["ptpa"], w=["idxT"])
                P.op("dve", lambda e, rt=rt: e.tensor_copy(out=self.gT[:, rt, :], in_=ptpb[:, 0:E]),
                     r=["ptpb"], w=["gT"])

    def phase_experts(self, l):
        c, P, nc = self.cfg, self.P, self.nc
        E, D, DC, FF, FC, CAP, CAPC = c.E, c.D, c.DC, c.FF, c.FC, c.CAP, c.CAPC
        R = CAP + CAPC
        RT = (R + 127) // 128
        with self.phase():
            zt = self.sb("zt", [128, D])
            Wg = self.sb("Wg", [128, DC, FF], BF16)
            Wu = self.sb("Wu", [128, DC, FF], BF16)
            Wd = self.sb("Wd", [128, FC, D], BF16)
            xg = self.sbn("xg", 2, [128, D], BF16)
            xsT = self.sb("xsT", [128, DC, 512], BF16)
            hT = self.sb("hT", [128, FC, 512], BF16)
            sg = self.sbn("sg", 2, [128, 512])
            yb = self.sbn("yb", 2, [128, D])
            ptr = self.psn("ptr", 2, [128, 1024], BF16)
            pg = self.psn("pg", 2, [128, 512])
            pu = self.psn("pu", 2, [128, 512])
            py = self.psn("py", 2, [128, 512])
            macc = self.scr["macc"].ap()
            mtok = self.scr["mtok"].ap()
            P.op("dve", lambda e: e.memset(zt[:], 0.0), w=["zt"])
            for r0 in range(0, c.NT + 128, 128):
                P.dma("sp", macc[r0:r0 + 128, :], zt[:], r=["zt"], w=["macc"])
            groups = [list(range(g, min(g + 4, RT))) for g in range(0, RT, 4)]
            for e_ in range(E):
                P.dma("pool", Wg[:], self.inp["moe_w_gate"].ap()[l, e_].rearrange("(c p) f -> p c f", p=128), w=["Wg"])
                P.dma("pool", Wu[:], self.inp["moe_w_up"].ap()[l, e_].rearrange("(c p) f -> p c f", p=128), w=["Wu"])
                P.dma("pool", Wd[:], self.inp["moe_w_down"].ap()[l, e_].rearrange("(c p) d -> p c d", p=128), w=["Wd"])
                for grp in groups:
                    gw = 0
                    offs = []
                    for rt in grp:
                        rows = 128
                        offs.append((rt, gw, rows))
                        gw += rows
                    for (rt, go, rows) in offs:
                        xi = self.rot("xg", 2)
                        xk = "xg%d" % xi
                        P.dma_fn("pool", lambda e, xi=xi, rt=rt, rows=rows, e_=e_: e.indirect_dma_start(
                            out=xg[xi][:rows, :], out_offset=None, in_=mtok,
                            in_offset=bass.IndirectOffsetOnAxis(ap=self.idxT[:rows, rt, e_:e_ + 1], axis=0)),
                            r=["mtok", "idxT"], w=[xk])
                        for c0 in range(0, DC, 8):
                            ti = self.rot("ptr", 2)
                            n8 = min(8, DC - c0)
                            for ci in range(n8):
                                P.op("pe", lambda e, ti=ti, ci=ci, c0=c0, xi=xi, rows=rows: e.transpose(
                                    ptr[ti][:, ci * 128:ci * 128 + rows],
                                    xg[xi][:rows, (c0 + ci) * 128:(c0 + ci + 1) * 128], self.id_b[:rows, :rows]),
                                    r=[xk, "id_b"], w=["ptr%d" % ti])
                            P.op("dve", lambda e, ti=ti, c0=c0, n8=n8, go=go, rows=rows: e.tensor_copy(
                                out=xsT[:, c0:c0 + n8, go:go + rows],
                                in_=ptr[ti][:, 0:n8 * 128].rearrange("p (c r) -> p c r", r=128)[:, :, :rows]),
                                r=["ptr%d" % ti], w=["xsT"])
                    for fc in range(FC):
                        gi = self.rot("pg", 2)
                        for cc in range(DC):
                            P.op("pe", lambda e, gi=gi, fc=fc, cc=cc, gw=gw: e.matmul(
                                pg[gi][:, :gw], Wg[:, cc, fc * 128:(fc + 1) * 128], xsT[:, cc, :gw],
                                start=(cc == 0), stop=(cc == DC - 1)), r=["Wg", "xsT"], w=["pg%d" % gi])
                        for cc in range(DC):
                            P.op("pe", lambda e, gi=gi, fc=fc, cc=cc, gw=gw: e.matmul(
                                pu[gi][:, :gw], Wu[:, cc, fc * 128:(fc + 1) * 128], xsT[:, cc, :gw],
                                start=(cc == 0), stop=(cc == DC - 1)), r=["Wu", "xsT"], w=["pu%d" % gi])
                        P.op("act", lambda e, gi=gi, gw=gw: e.activation(out=sg[gi][:, :gw], in_=pg[gi][:, :gw],
                                                                        func=AF.Silu), r=["pg%d" % gi], w=["sg%d" % gi])
                        P.op("dve", lambda e, gi=gi, gw=gw, fc=fc: e.tensor_tensor(
                            out=hT[:, fc, :gw], in0=sg[gi][:, :gw], in1=pu[gi][:, :gw], op=ALU.mult),
                            r=["sg%d" % gi, "pu%d" % gi], w=["hT%d" % fc])
                    for (rt, go, rows) in offs:
                        yi = self.rot("yb", 2)
                        yk = "yb%d" % yi
                        for j in range(D // 512):
                            pi = self.rot("py", 2)
                            for fc in range(FC):
                                P.op("pe", lambda e, pi=pi, fc=fc, go=go, rows=rows, j=j: e.matmul(
                                    py[pi][:rows, :], hT[:, fc, go:go + rows], Wd[:, fc, j * 512:(j + 1) * 512],
                                    start=(fc == 0), stop=(fc == FC - 1)), r=["hT%d" % fc, "Wd"], w=["py%d" % pi])
                            P.op("act", lambda e, pi=pi, yi=yi, rows=rows, j=j, rt=rt, e_=e_: e.activation(
                                out=yb[yi][:rows, j * 512:(j + 1) * 512], in_=py[pi][:rows, :], func=AF.Copy,
                                scale=self.gT[:rows, rt, e_:e_ + 1]), r=["py%d" % pi, "gT"], w=[yk + "_%d" % j])
                        P.dma_fn("pool", lambda e, yi=yi, rt=rt, rows=rows, e_=e_: e.indirect_dma_start(
                            out=macc, out_offset=bass.IndirectOffsetOnAxis(ap=self.idxT[:rows, rt, e_:e_ + 1], axis=0),
                            in_=yb[yi][:rows, :], in_offset=None, compute_op=ALU.add),
                            r=[yk + "_%d" % j for j in range(D // 512)] + ["idxT"], w=["macc"])

    def phase_combine(self, l):
        c, P = self.cfg, self.P
        DC, D = c.DC, c.D
        with self.phase():
            xin = self.sbn("xin", 2, [128, DC, 512])
            mc = self.sbn("mc", 2, [128, 4, D])
            pc = self.psn("pc", 4, [128, 512])
            xv = self.scr["xT"].ap().rearrange("(c p) n -> p c n", p=128)
            macc = self.scr["macc"].ap()
            for (t0, tw, v) in c.tiles():
                bi = self.rot("xin", 2)
                xk, mk = "xin%d" % bi, "mc%d" % bi
                ns = tw // 128
                P.dma("sp", xin[bi][:, :, :tw], xv[:, :, t0:t0 + tw], r=["xT"], w=[xk])
                P.dma("sp", mc[bi][:, :ns, :], macc[t0:t0 + tw, :].rearrange("(s p) d -> p s d", p=128),
                      r=["macc"], w=[mk])
                for cc in range(DC):
                    pi = self.rot("pc", 4)
                    for s_ in range(ns):
                        P.op("pe", lambda e, pi=pi, s_=s_, cc=cc, bi=bi: e.transpose(
                            pc[pi][:, s_ * 128:(s_ + 1) * 128], mc[bi][:, s_, cc * 128:(cc + 1) * 128], self.id_f[:, :]),
                            r=[mk, "id_f"], w=["pc%d" % pi])
                    P.op("dve", lambda e, pi=pi, cc=cc, bi=bi, tw=tw, v=v: e.scalar_tensor_tensor(
                        out=xin[bi][:, cc, :tw], in0=pc[pi][:, :tw], scalar=self.modcol(l, v, 5, cc),
                        in1=xin[bi][:, cc, :tw], op0=ALU.mult, op1=ALU.add), r=["pc%d" % pi, xk, "modv"], w=[xk])
                P.dma("act", xv[:, :, t0:t0 + tw], xin[bi][:, :, :tw], r=[xk], w=["xT"])

    def moe(self, l):
        self.phase_norm2_router(l)
        self.phase_tok()
        self.phase_topk(l)
        self.phase_experts(l)
        self.phase_combine(l)

    def phase_pool(self, l):
        c, P = self.cfg, self.P
        DC, D, PGC, PG, NT, S = c.DC, c.D, c.PGC, c.PG, c.NT, c.S
        HW = 8
        with self.phase():
            pw = self.sb("pw", [128, 4, PGC, PG], BF16)
            psc = self.sb("psc", [128, DC])
            sc = self.sb("sc", [128, 2, DC])
            nw = self.sbn("nw", 2, [128, DC, 512 + 2 * HW], BF16)
            invb = self.sbn("invb", 2, [128, 4, 512])
            ta = self.sbn("ta", 2, [128, 512 + 2 * HW])
            tb = self.sbn("tb", 2, [128, 512 + 2 * HW])
            dT = self.sbn("dT", 2, [128, DC, 512], BF16)
            xin = self.sbn("xin", 2, [128, DC, 512])
            pp = self.psn("ppool", 4, [128, 512])
            for g in range(4):
                P.dma("pool", pw[:, g], self.inp["pool_w"].ap()[g].rearrange("(k p) o -> p k o", p=128), w=["pw"])
            P.dma("sp", psc[:], self.inp["pool_scaleT"].ap(), w=["psc"])
            for v in range(2):
                P.op("dve", lambda e, v=v: e.tensor_tensor(out=sc[:, v, :], in0=psc[:], in1=self.modv[:, l, v, 2 * DC:3 * DC],
                                                          op=ALU.mult), r=["psc", "modv"], w=["sc"])
            nv = self.scr["nT"].ap().rearrange("(c p) n -> p c n", p=128)
            xv = self.scr["xT"].ap().rearrange("(c p) n -> p c n", p=128)
            invt = self.inp["pool_inv"]
            for (t0, tw, v) in c.tiles():
                lo_seq, hi_seq = (0, S) if v == 0 else (S, NT)
                bi = self.rot("nw", 2)
                nk, ik, dk, xk = "nw%d" % bi, "invb%d" % bi, "dT%d" % bi, "xin%d" % bi
                W = tw + 2 * HW
                a0, a1 = max(t0 - HW, lo_seq), min(t0 + tw + HW, hi_seq)
                P.op("pool", lambda e, bi=bi: e.memset(nw[bi][:], 0.0), w=[nk])
                P.dma("sp", nw[bi][:, :, a0 - (t0 - HW):a1 - (t0 - HW)], nv[:, :, a0:a1], r=["nT"], w=[nk])
                P.dma("sp", invb[bi][:, :, :tw], bass.AP(invt, t0, [[0, 128], [NT, 4], [1, tw]]), w=[ik])
                P.dma("sp", xin[bi][:, :, :tw], xv[:, :, t0:t0 + tw], r=["xT"], w=[xk])
                for cc in range(DC):
                    g = cc // PGC
                    ti = self.rot("ta", 2)
                    A, Bf = ta[ti], tb[ti]
                    ak, bk = "ta%d" % ti, "tb%d" % ti
                    src = nw[bi][:, cc, :]
                    P.op("dve", lambda e, A=A, src=src, W=W: e.tensor_tensor(out=A[:, 1:W], in0=src[:, 0:W - 1], in1=src[:, 1:W],
                                                                           op=ALU.add), r=[nk], w=[ak])
                    cur, curk, oth, othk = A, ak, Bf, bk
                    lo, hi, sh = 1, W, 1
                    for step in range(g):
                        nlo, nhi = lo + sh, hi - sh
                        P.op("dve", lambda e, cur=cur, oth=oth, nlo=nlo, nhi=nhi, sh=sh: e.tensor_tensor(
                            out=oth[:, nlo:nhi], in0=cur[:, nlo - sh:nhi - sh], in1=cur[:, nlo + sh:nhi + sh], op=ALU.add),
                            r=[curk], w=[othk])
                        cur, curk, oth, othk = oth, othk, cur, curk
                        lo, hi, sh = nlo, nhi, sh * 2
                    P.op("dve", lambda e, cur=cur, oth=oth, bi=bi, g=g, tw=tw: e.tensor_tensor(
                        out=oth[:, HW:HW + tw], in0=cur[:, HW:HW + tw], in1=invb[bi][:, g, :tw], op=ALU.mult),
                        r=[curk, ik], w=[othk])
                    P.op("dve", lambda e, oth=oth, bi=bi, cc=cc, tw=tw, src=src: e.tensor_tensor(
                        out=dT[bi][:, cc, :tw], in0=oth[:, HW:HW + tw], in1=src[:, HW:HW + tw], op=ALU.subtract),
                        r=[othk, nk], w=[dk + "_%d" % cc])
                for f in range(DC):
                    g, fl = divmod(f, PGC)
                    pi = self.rot("ppool", 4)
                    for kc in range(PGC):
                        P.op("pe", lambda e, pi=pi, g=g, kc=kc, fl=fl, bi=bi, tw=tw: e.matmul(
                            pp[pi][:, :tw], pw[:, g, kc, fl * 128:(fl + 1) * 128], dT[bi][:, g * PGC + kc, :tw],
                            start=(kc == 0), stop=(kc == PGC - 1)), r=["pw", dk + "_%d" % (g * PGC + kc)], w=["ppool%d" % pi])
                    P.op("dve", lambda e, pi=pi, f=f, bi=bi, tw=tw, v=v: e.scalar_tensor_tensor(
                        out=xin[bi][:, f, :tw], in0=pp[pi][:, :tw], scalar=sc[:, v, f:f + 1], in1=xin[bi][:, f, :tw],
                        op0=ALU.mult, op1=ALU.add), r=["ppool%d" % pi, "sc", xk], w=[xk])
                P.dma("act", xv[:, :, t0:t0 + tw], xin[bi][:, :, :tw], r=[xk], w=["xT"])

    def phase_linear_res(self, l, wname, bias_name=None, src="nT", ctx=True, wsel=None):
        c, P = self.cfg, self.P
        DC, D = c.DC, c.D
        with self.phase():
            W = self.sb("Wl", [128, DC, D], BF16)
            hin = self.sbn("hin", 2, [128, DC, 512], BF16)
            xin = self.sbn("xin", 2, [128, DC, 512])
            tb = self.sbn("lt", 2, [128, 512])
            pp = self.psn("pl", 4, [128, 512])
            wap = self.inp[wname].ap()
            for c0 in range(0, DC, 4):
                P.dma("pool", W[:, c0:c0 + 4, :], wap.rearrange("(c p) o -> p c o", p=128)[:, c0:c0 + 4, :], w=["Wl"])
            if bias_name:
                bs = self.sb("bs", [128, DC])
                P.dma("sp", bs[:], self.inp[bias_name].ap(), w=["bs"])
            hv = self.scr[src].ap().rearrange("(c p) n -> p c n", p=128)
            xv = self.scr["xT"].ap().rearrange("(c p) n -> p c n", p=128)
            for (t0, tw, v) in c.tiles(ctx=ctx):
                bi = self.rot("hin", 2)
                hk, xk = "hin%d" % bi, "xin%d" % bi
                P.dma("sp", hin[bi][:, :, :tw], hv[:, :, t0:t0 + tw], r=[src], w=[hk])
                P.dma("sp", xin[bi][:, :, :tw], xv[:, :, t0:t0 + tw], r=["xT"], w=[xk])
                for f in range(DC):
                    pi = self.rot("pl", 4)
                    for cc in range(DC):
                        P.op("pe", lambda e, pi=pi, f=f, cc=cc, bi=bi, tw=tw: e.matmul(
                            pp[pi][:, :tw], W[:, cc, f * 128:(f + 1) * 128], hin[bi][:, cc, :tw],
                            start=(cc == 0), stop=(cc == DC - 1)), r=["Wl", hk], w=["pl%d" % pi])
                    if bias_name:
                        ti = self.rot("lt", 2)
                        P.op("act", lambda e, pi=pi, ti=ti, f=f, tw=tw: e.activation(
                            out=tb[ti][:, :tw], in_=pp[pi][:, :tw], func=AF.Identity, scale=1.0, bias=bs[:, f:f + 1]),
                            r=["pl%d" % pi, "bs"], w=["lt%d" % ti])
                        P.op("dve", lambda e, ti=ti, f=f, bi=bi, tw=tw, v=v: e.scalar_tensor_tensor(
                            out=xin[bi][:, f, :tw], in0=tb[ti][:, :tw], scalar=self.modcol(l, v, 2, f),
                            in1=xin[bi][:, f, :tw], op0=ALU.mult, op1=ALU.add), r=["lt%d" % ti, "modv", xk], w=[xk])
                    else:
                        P.op("dve", lambda e, pi=pi, f=f, bi=bi, tw=tw, v=v: e.scalar_tensor_tensor(
                            out=xin[bi][:, f, :tw], in0=pp[pi][:, :tw], scalar=self.modcol(l, v, 2, f),
                            in1=xin[bi][:, f, :tw], op0=ALU.mult, op1=ALU.add), r=["pl%d" % pi, "modv", xk], w=[xk])
                P.dma("act", xv[:, :, t0:t0 + tw], xin[bi][:, :, :tw], r=[xk], w=["xT"])

    def phase_conv1(self, l):
        c, P = self.cfg, self.P
        DC, D = c.DC, c.D
        HC = DC // 2
        with self.phase():
            Wa = self.sb("Wa", [128, DC, HC * 128], BF16)
            Wg = self.sb("Wgt", [128, DC, HC * 128], BF16)
            b1 = self.sb("b1", [128, 2 * DC])
            nin = self.sbn("nin", 2, [128, DC, 512], BF16)
            sgm = self.sbn("sgm", 2, [128, 512])
            ub = self.sbn("ub", 2, [128, HC, 512])
            pa = self.psn("pa", 2, [128, 512])
            pg = self.psn("pgt", 2, [128, 512])
            wv = self.inp["conv_pw1_w"].ap().rearrange("(c p) o -> p c o", p=128)
            nv = self.scr["nT"].ap().rearrange("(c p) n -> p c n", p=128)
            uv = self.scr["uT"].ap().rearrange("(c p) n -> p c n", p=128)
            P.dma("sp", b1[:], self.inp["pw1_bT"].ap(), w=["b1"])
            for half in range(2):
                f0 = half * HC
                for c0 in range(0, DC, 4):
                    P.dma("pool", Wa[:, c0:c0 + 4, :], wv[:, c0:c0 + 4, f0 * 128:(f0 + HC) * 128], w=["Wa"])
                    P.dma("pool", Wg[:, c0:c0 + 4, :], wv[:, c0:c0 + 4, D + f0 * 128:D + (f0 + HC) * 128], w=["Wgt"])
                for (t0, tw, v) in c.tiles():
                    bi = self.rot("nin", 2)
                    nk, uk = "nin%d" % bi, "ub%d" % bi
                    P.dma("sp", nin[bi][:, :, :tw], nv[:, :, t0:t0 + tw], r=["nT"], w=[nk])
                    for fl in range(HC):
                        f = f0 + fl
                        pi = self.rot("pa", 2)
                        for cc in range(DC):
                            P.op("pe", lambda e, pi=pi, fl=fl, cc=cc, bi=bi, tw=tw: e.matmul(
                                pa[pi][:, :tw], Wa[:, cc, fl * 128:(fl + 1) * 128], nin[bi][:, cc, :tw],
                                start=(cc == 0), stop=(cc == DC - 1)), r=["Wa", nk], w=["pa%d" % pi])
                        for cc in range(DC):
                            P.op("pe", lambda e, pi=pi, fl=fl, cc=cc, bi=bi, tw=tw: e.matmul(
                                pg[pi][:, :tw], Wg[:, cc, fl * 128:(fl + 1) * 128], nin[bi][:, cc, :tw],
                                start=(cc == 0), stop=(cc == DC - 1)), r=["Wgt", nk], w=["pgt%d" % pi])
                        P.op("act", lambda e, pi=pi, f=f, tw=tw: e.activation(
                            out=sgm[pi][:, :tw], in_=pg[pi][:, :tw], func=AF.Sigmoid, scale=1.0,
                            bias=b1[:, DC + f:DC + f + 1]), r=["pgt%d" % pi, "b1"], w=["sgm%d" % pi])
                        P.op("dve", lambda e, pi=pi, f=f, fl=fl, bi=bi, tw=tw: e.scalar_tensor_tensor(
                            out=ub[bi][:, fl, :tw], in0=pa[pi][:, :tw], scalar=b1[:, f:f + 1], in1=sgm[pi][:, :tw],
                            op0=ALU.add, op1=ALU.mult), r=["pa%d" % pi, "sgm%d" % pi, "b1"], w=[uk + "_%d" % fl])
                    P.dma("act", uv[:, f0:f0 + HC, t0:t0 + tw], ub[bi][:, :, :tw],
                          r=[uk + "_%d" % fl for fl in range(HC)], w=["uT"])

    def phase_conv2(self, l):
        c, P = self.cfg, self.P
        DC, D, NT, S, KW = c.DC, c.D, c.NT, c.S, c.CONVW
        HW = KW // 2
        with self.phase():
            dw = self.sb("dw", [128, DC, KW])
            dwb = self.sb("dwb", [128, DC])
            lg = self.sb("lg", [128, DC])
            lb = self.sb("lb", [128, DC])
            uw = self.sbn("uw", 2, [128, DC, 512 + 2 * HW])
            vv = self.sb("vv", [128, DC, 512])
            sqt = self.sbn("sqt", 2, [128, 512])
            mu = self.sb("mu", [128, 512])
            msq = self.sb("msq", [128, 512])
            rstd = self.sb("rstd", [128, 512])
            t1 = self.sbn("t1", 2, [128, 512])
            hb = self.sbn("hb", 2, [128, DC, 512], BF16)
            pm = self.ps("pmn", [128, 512])
            p2 = self.ps("pm2", [128, 512])
            I = self.inp
            P.dma("sp", dw[:], I["dw_wT"].ap(), w=["dw"])
            P.dma("sp", dwb[:], I["dw_bT"].ap(), w=["dwb"])
            P.dma("sp", lg[:], I["ln_gT"].ap(), w=["lg"])
            P.dma("sp", lb[:], I["ln_bT"].ap(), w=["lb"])
            uv = self.scr["uT"].ap().rearrange("(c p) n -> p c n", p=128)
            nv = self.scr["nT"].ap().rearrange("(c p) n -> p c n", p=128)
            for (t0, tw, v) in c.tiles():
                lo_seq, hi_seq = (0, S) if v == 0 else (S, NT)
                bi = self.rot("uw", 2)
                uk, hk = "uw%d" % bi, "hb%d" % bi
                a0, a1 = max(t0 - HW, lo_seq), min(t0 + tw + HW, hi_seq)
                if a0 > t0 - HW or a1 < t0 + tw + HW:
                    P.op("pool", lambda e, bi=bi: e.memset(uw[bi][:], 0.0), w=[uk])
                P.dma("sp", uw[bi][:, :, a0 - (t0 - HW):a1 - (t0 - HW)], uv[:, :, a0:a1], r=["uT"], w=[uk])
                for cc in range(DC):
                    P.op("dve", lambda e, bi=bi, cc=cc, tw=tw: e.tensor_scalar(
                        out=vv[:, cc, :tw], in0=uw[bi][:, cc, 0:tw], scalar1=dw[:, cc, 0:1], scalar2=dwb[:, cc:cc + 1],
                        op0=ALU.mult, op1=ALU.add), r=[uk, "dw", "dwb"], w=["vv%d" % cc])
                    for k in range(1, KW):
                        P.op("dve", lambda e, bi=bi, cc=cc, tw=tw, k=k: e.scalar_tensor_tensor(
                            out=vv[:, cc, :tw], in0=uw[bi][:, cc, k:k + tw], scalar=dw[:, cc, k:k + 1], in1=vv[:, cc, :tw],
                            op0=ALU.mult, op1=ALU.add), r=[uk, "dw"], w=["vv%d" % cc])
                    si = self.rot("sqt", 2)
                    P.op("act", lambda e, si=si, cc=cc, tw=tw: e.activation(out=sqt[si][:, :tw], in_=vv[:, cc, :tw],
                                                                          func=AF.Square), r=["vv%d" % cc], w=["sqt%d" % si])
                    P.op("pe", lambda e, cc=cc, tw=tw: e.matmul(pm[:, :tw], self.ones_f[:, :], vv[:, cc, :tw],
                                                              start=(cc == 0), stop=(cc == DC - 1)),
                         r=["vv%d" % cc, "ones_f"], w=["pmn"])
                    P.op("pe", lambda e, cc=cc, tw=tw, si=si: e.matmul(p2[:, :tw], self.ones_f[:, :], sqt[si][:, :tw],
                                                                     start=(cc == 0), stop=(cc == DC - 1)),
                         r=["sqt%d" % si, "ones_f"], w=["pm2"])
                P.op("dve", lambda e, tw=tw: e.tensor_scalar(out=mu[:, :tw], in0=pm[:, :tw], scalar1=1.0 / D, scalar2=None,
                                                            op0=ALU.mult), r=["pmn"], w=["mu"])
                P.op("dve", lambda e, tw=tw: e.tensor_tensor(out=msq[:, :tw], in0=mu[:, :tw], in1=mu[:, :tw], op=ALU.mult),
                     r=["mu"], w=["msq"])
                P.op("dve", lambda e, tw=tw: e.scalar_tensor_tensor(
                    out=msq[:, :tw], in0=p2[:, :tw], scalar=1.0 / D, in1=msq[:, :tw], op0=ALU.mult, op1=ALU.subtract),
                    r=["pm2", "msq"], w=["msq"])
                self.rsqrt(rstd[:, :tw], msq[:, :tw], 1.0, r=["msq"], w=["rstd"])
                for cc in range(DC):
                    ti = self.rot("t1", 2)
                    P.op("dve", lambda e, ti=ti, cc=cc, tw=tw: e.tensor_tensor(
                        out=t1[ti][:, :tw], in0=vv[:, cc, :tw], in1=mu[:, :tw], op=ALU.subtract),
                        r=["vv%d" % cc, "mu"], w=["t1%d" % ti])
                    P.op("dve", lambda e, ti=ti, tw=tw: e.tensor_tensor(
                        out=t1[ti][:, :tw], in0=t1[ti][:, :tw], in1=rstd[:, :tw], op=ALU.mult),
                        r=["t1%d" % ti, "rstd"], w=["t1%d" % ti])
                    P.op("act", lambda e, ti=ti, cc=cc, bi=bi, tw=tw: e.activation(
                        out=hb[bi][:, cc, :tw], in_=t1[ti][:, :tw], func=AF.Silu, scale=lg[:, cc:cc + 1],
                        bias=lb[:, cc:cc + 1]), r=["t1%d" % ti, "lg", "lb"], w=[hk + "_%d" % cc])
                P.dma("act", nv[:, :, t0:t0 + tw], hb[bi][:, :, :tw], r=[hk + "_%d" % cc for cc in range(DC)], w=["nT"])

    def conv_mixer(self, l):
        self.phase_norm(l, 0)
        self.phase_conv1(l)
        self.phase_conv2(l)
        self.phase_linear_res(l, "conv_pw2_w", "pw2_bT")

    def rms_chunks(self, srcf, dst, nchunk, dim, tw, gcol, pst, rs, sq, pfx):
        P = self.P
        P.op("act", lambda e: e.activation(out=sq[:, :nchunk, :tw], in_=srcf[:, :nchunk, :tw], func=AF.Square),
             r=[pfx + "f%d" % q for q in range(nchunk)], w=[pfx + "sq"])
        for q in range(nchunk):
            P.op("pe", lambda e, q=q: e.matmul(pst[:, :tw], self.ones_f[:, :], sq[:, q, :tw], start=(q == 0),
                                               stop=(q == nchunk - 1)), r=[pfx + "sq", "ones_f"], w=[pfx + "pst"])
        self.rsqrt(rs[:, :tw], pst[:, :tw], 1.0 / dim, r=[pfx + "pst"], w=[pfx + "rs"])
        for q in range(nchunk):
            P.op("dve", lambda e, q=q: e.tensor_tensor(out=sq[:, q, :tw], in0=srcf[:, q, :tw], in1=rs[:, :tw], op=ALU.mult),
                 r=[pfx + "f%d" % q, pfx + "rs", pfx + "sq"], w=[pfx + "sq%d" % q])
            P.op("act", lambda e, q=q: e.activation(out=dst[:, q, :tw], in_=sq[:, q, :tw], func=AF.Identity,
                                                    scale=gcol(q), bias=self.zero_c[:, 0:1]),
                 r=[pfx + "sq%d" % q], w=[pfx + "n%d" % q])

    def phase_mla_q(self, l, h0, H):
        c, P = self.cfg, self.P
        DC, QC, QR, NT = c.DC, c.QC, c.QR, c.NT
        with self.phase():
            Wdq = self.sb("Wdq", [128, DC, QR], BF16)
            Wuq = self.sb("Wuq", [128, QC, H * 192], BF16)
            Wsw = self.sb("Wsw", [128, QC, H, 64], BF16)
            qn_ = self.sb("qnrm", [128, QC])
            nin = self.sbn("nin", 2, [128, DC, 512], BF16)
            cqf = self.sb("cqf", [128, QC, 512])
            sq = self.sb("cqsq", [128, QC, 512])
            rs = self.sb("cqrs", [128, 512])
            cqn = self.sb("cqn", [128, QC, 512], BF16)
            rt = self.sbn("rt", 2, [64, 2, 512])
            ta = self.sbn("rta", 2, [64, 512])
            tb = self.sbn("rtb", 2, [64, 512])
            qna = self.sbn("qna", 2, [128, H, 512], BF16)
            qra = self.sbn("qra", 2, [64, H, 512], BF16)
            pc = self.psn("pcq", 2, [128, 512])
            pq = self.psn("pq", 2, [128, 512])
            p1 = self.psn("pr1", 2, [128, 512])
            p2 = self.psn("pr2", 2, [128, 512])
            I = self.inp
            P.dma("sp", qn_[:], I["q_normT"].ap(), w=["qnrm"])
            wdq = I["mla_w_dq"].ap().rearrange("(c p) o -> p c o", p=128)
            for c0 in range(0, DC, 4):
                P.dma("pool", Wdq[:, c0:c0 + 4, :], wdq[:, c0:c0 + 4, :], w=["Wdq"])
            wuq = I["mla_w_uq"].ap().rearrange("(c p) o -> p c o", p=128)
            wuq4 = I["mla_w_uq"].ap().rearrange("(c p) (h x) -> p c h x", p=128, x=192)
            for qc in range(QC):
                P.dma("pool", Wuq[:, qc, :], wuq[:, qc, h0 * 192:(h0 + H) * 192], w=["Wuq"])
                P.dma("pool", Wsw[:, qc, :, 0:32], wuq4[:, qc, h0:h0 + H, 160:192], w=["Wsw"])
                P.dma("pool", Wsw[:, qc, :, 32:64], wuq4[:, qc, h0:h0 + H, 128:160], w=["Wsw"])
            nv = self.scr["nT"].ap().rearrange("(c p) n -> p c n", p=128)
            ropev = I["rope_mla"].ap().rearrange("a p n -> p a n")
            for (t0, tw, v) in c.tiles():
                bi = self.rot("nin", 2)
                nk, rk, qk, qrk = "nin%d" % bi, "rt%d" % bi, "qna%d" % bi, "qra%d" % bi
                P.dma("sp", nin[bi][:, :, :tw], nv[:, :, t0:t0 + tw], r=["nT"], w=[nk])
                P.dma("sp", rt[bi][:, :, :tw], ropev[:, :, t0:t0 + tw], w=[rk])
                for qc in range(QC):
                    pi = self.rot("pcq", 2)
                    for cc in range(DC):
                        P.op("pe", lambda e, pi=pi, qc=qc, cc=cc, bi=bi, tw=tw: e.matmul(
                            pc[pi][:, :tw], Wdq[:, cc, qc * 128:(qc + 1) * 128], nin[bi][:, cc, :tw],
                            start=(cc == 0), stop=(cc == DC - 1)), r=["Wdq", nk], w=["pcq%d" % pi])
                    P.op("act", lambda e, pi=pi, qc=qc, tw=tw: e.activation(out=cqf[:, qc, :tw], in_=pc[pi][:, :tw],
                                                                          func=AF.Copy), r=["pcq%d" % pi], w=["cqf%d" % qc])
                pst = pc[self.rot("pcq", 2)]
                self.rms_chunks(cqf, cqn, QC, QR, tw, lambda q: qn_[:, q:q + 1], pst, rs, sq, "cq")
                cqk = ["cqn%d" % q for q in range(QC)]
                for h in range(H):
                    pi = self.rot("pq", 2)
                    for qc in range(QC):
                        P.op("pe", lambda e, pi=pi, qc=qc, h=h, tw=tw: e.matmul(
                            pq[pi][:, :tw], Wuq[:, qc, h * 192:h * 192 + 128], cqn[:, qc, :tw],
                            start=(qc == 0), stop=(qc == QC - 1)), r=["Wuq"] + cqk, w=["pq%d" % pi])
                    for qc in range(QC):
                        P.op("pe", lambda e, pi=pi, qc=qc, h=h, tw=tw: e.matmul(
                            p1[pi][0:64, :tw], Wuq[:, qc, h * 192 + 128:h * 192 + 192], cqn[:, qc, :tw],
                            start=(qc == 0), stop=(qc == QC - 1)), r=["Wuq"] + cqk, w=["pr1%d" % pi])
                    for qc in range(QC):
                        P.op("pe", lambda e, pi=pi, qc=qc, h=h, tw=tw: e.matmul(
                            p2[pi][0:64, :tw], Wsw[:, qc, h, :], cqn[:, qc, :tw],
                            start=(qc == 0), stop=(qc == QC - 1)), r=["Wsw"] + cqk, w=["pr2%d" % pi])
                    P.op("act", lambda e, pi=pi, h=h, bi=bi, tw=tw: e.activation(out=qna[bi][:, h, :tw], in_=pq[pi][:, :tw],
                                                                                func=AF.Copy), r=["pq%d" % pi], w=[qk + "_%d" % h])
                    P.op("dve", lambda e, pi=pi, bi=bi, tw=tw: e.tensor_tensor(
                        out=ta[pi][:, :tw], in0=p1[pi][0:64, :tw], in1=rt[bi][:, 0, :tw], op=ALU.mult),
                        r=["pr1%d" % pi, rk], w=["rta%d" % pi])
                    P.op("dve", lambda e, pi=pi, bi=bi, tw=tw: e.tensor_tensor(
                        out=tb[pi][:, :tw], in0=p2[pi][0:64, :tw], in1=rt[bi][:, 1, :tw], op=ALU.mult),
                        r=["pr2%d" % pi, rk], w=["rtb%d" % pi])
                    P.op("pool", lambda e, pi=pi, bi=bi, h=h, tw=tw: e.tensor_tensor(
                        out=qra[bi][:, h, :tw], in0=ta[pi][:, :tw], in1=tb[pi][:, :tw], op=ALU.add),
                        r=["rta%d" % pi, "rtb%d" % pi], w=[qrk + "_%d" % h])
                P.dma("act", self.scr["qnT"].ap()[:, h0:h0 + H, t0:t0 + tw], qna[bi][:, :, :tw],
                      r=[qk + "_%d" % h for h in range(H)], w=["qnT"])
                P.dma("act", self.scr["qrT"].ap()[:, h0:h0 + H, t0:t0 + tw], qra[bi][:, :, :tw],
                      r=[qrk + "_%d" % h for h in range(H)], w=["qrT"])

    def phase_mla_kv(self, l):
        c, P = self.cfg, self.P
        DC, KVC, KVR, H, NT = c.DC, c.KVC, c.KVR, c.H, c.NT
        HV = H * 128
        with self.phase():
            Wd = self.sb("Wdkv", [128, DC, KVR + 64], BF16)
            Wks = self.sb("Wkrs", [128, DC, 64], BF16)
            Wk = self.sb("Wk", [128, KVC, H, 128], BF16)
            Wv = self.sb("Wv", [128, KVC, H, 128], BF16)
            kvn_ = self.sb("kvnrm", [128, KVC])
            nin = self.sbn("nin", 2, [128, DC, 512], BF16)
            ckf = self.sb("ckf", [128, KVC, 512])
            sq = self.sb("cksq", [128, KVC, 512])
            rs = self.sb("ckrs", [128, 512])
            ckn = self.sb("ckn", [128, KVC, 512], BF16)
            rt = self.sbn("rt", 2, [64, 2, 512])
            ta = self.sb("rta", [64, 512])
            tb = self.sb("rtb", [64, 512])
            krs = self.sbn("krs", 2, [64, 512], BF16)
            kna = self.sbn("kna", 2, [128, H, 512], BF16)
            vt = self.sbn("vt", 2, [128, HV], BF16)
            pk = self.psn("pkv", 2, [128, 512])
            p1 = self.ps("pr1", [128, 512])
            p2 = self.ps("pr2", [128, 512])
            pn = self.psn("pkn", 2, [128, 512])
            pv = self.psn("pv", 2, [128, 512])
            I = self.inp
            P.dma("sp", kvn_[:], I["kv_normT"].ap(), w=["kvnrm"])
            wd = I["mla_w_dkv"].ap().rearrange("(c p) o -> p c o", p=128)
            for c0 in range(0, DC, 4):
                P.dma("pool", Wd[:, c0:c0 + 4, :], wd[:, c0:c0 + 4, :], w=["Wdkv"])
            P.dma("pool", Wks[:, :, 0:32], wd[:, :, KVR + 32:KVR + 64], w=["Wkrs"])
            P.dma("pool", Wks[:, :, 32:64], wd[:, :, KVR:KVR + 32], w=["Wkrs"])
            wu4 = I["mla_w_ukv"].ap().rearrange("(c p) (h x) -> p c h x", p=128, x=256)
            for kc in range(KVC):
                P.dma("pool", Wk[:, kc, :, :], wu4[:, kc, :, 0:128], w=["Wk"])
                P.dma("pool", Wv[:, kc, :, :], wu4[:, kc, :, 128:256], w=["Wv"])
            nv = self.scr["nT"].ap().rearrange("(c p) n -> p c n", p=128)
            ropev = I["rope_mla"].ap().rearrange("a p n -> p a n")
            for (t0, tw, v) in c.tiles():
                bi = self.rot("nin", 2)
                nk, rk, kk, krk = "nin%d" % bi, "rt%d" % bi, "kna%d" % bi, "krs%d" % bi
                P.dma("sp", nin[bi][:, :, :tw], nv[:, :, t0:t0 + tw], r=["nT"], w=[nk])
                P.dma("sp", rt[bi][:, :, :tw], ropev[:, :, t0:t0 + tw], w=[rk])
                for kc in range(KVC):
                    pi = self.rot("pkv", 2)
                    for cc in range(DC):
                        P.op("pe", lambda e, pi=pi, kc=kc, cc=cc, bi=bi, tw=tw: e.matmul(
                            pk[pi][:, :tw], Wd[:, cc, kc * 128:(kc + 1) * 128], nin[bi][:, cc, :tw],
                            start=(cc == 0), stop=(cc == DC - 1)), r=["Wdkv", nk], w=["pkv%d" % pi])
                    P.op("act", lambda e, pi=pi, kc=kc, tw=tw: e.activation(out=ckf[:, kc, :tw], in_=pk[pi][:, :tw],
                                                                          func=AF.Copy), r=["pkv%d" % pi], w=["ckf%d" % kc])
                for cc in range(DC):
                    P.op("pe", lambda e, cc=cc, bi=bi, tw=tw: e.matmul(
                        p1[0:64, :tw], Wd[:, cc, KVR:KVR + 64], nin[bi][:, cc, :tw], start=(cc == 0), stop=(cc == DC - 1)),
                        r=["Wdkv", nk], w=["pr1"])
                for cc in range(DC):
                    P.op("pe", lambda e, cc=cc, bi=bi, tw=tw: e.matmul(
                        p2[0:64, :tw], Wks[:, cc, :], nin[bi][:, cc, :tw], start=(cc == 0), stop=(cc == DC - 1)),
                        r=["Wkrs", nk], w=["pr2"])
                P.op("dve", lambda e, bi=bi, tw=tw: e.tensor_tensor(out=ta[:, :tw], in0=p1[0:64, :tw], in1=rt[bi][:, 0, :tw],
                                                                   op=ALU.mult), r=["pr1", rk], w=["rta"])
                P.op("dve", lambda e, bi=bi, tw=tw: e.tensor_tensor(out=tb[:, :tw], in0=p2[0:64, :tw], in1=rt[bi][:, 1, :tw],
                                                                   op=ALU.mult), r=["pr2", rk], w=["rtb"])
                P.op("pool", lambda e, bi=bi, tw=tw: e.tensor_tensor(out=krs[bi][:, :tw], in0=ta[:, :tw], in1=tb[:, :tw],
                                                                    op=ALU.add), r=["rta", "rtb"], w=[krk])
                P.dma("act", self.scr["krT"].ap()[:, t0:t0 + tw], krs[bi][:, :tw], r=[krk], w=["krT"])
                pst = pk[self.rot("pkv", 2)]
                self.rms_chunks(ckf, ckn, KVC, KVR, tw, lambda q: kvn_[:, q:q + 1], pst, rs, sq, "ck")
                ckk = ["ckn%d" % q for q in range(KVC)]
                for h in range(H):
                    pi = self.rot("pkn", 2)
                    for kc in range(KVC):
                        P.op("pe", lambda e, pi=pi, kc=kc, h=h, tw=tw: e.matmul(
                            pn[pi][:, :tw], Wk[:, kc, h, :], ckn[:, kc, :tw], start=(kc == 0), stop=(kc == KVC - 1)),
                            r=["Wk"] + ckk, w=["pkn%d" % pi])
                    if h % 2 == 0:
                        P.op("act", lambda e, pi=pi, h=h, bi=bi, tw=tw: e.activation(
                            out=kna[bi][:, h, :tw], in_=pn[pi][:, :tw], func=AF.Copy), r=["pkn%d" % pi], w=[kk + "_%d" % h])
                    else:
                        P.op("dve", lambda e, pi=pi, h=h, bi=bi, tw=tw: e.tensor_copy(
                            out=kna[bi][:, h, :tw], in_=pn[pi][:, :tw]), r=["pkn%d" % pi], w=[kk + "_%d" % h])
                P.dma("act", self.scr["knT"].ap()[:, 0:H, t0:t0 + tw], kna[bi][:, :, :tw],
                      r=[kk + "_%d" % h for h in range(H)], w=["knT"])
                for ts in range(tw // 128):
                    vi = self.rot("vt", 2)
                    vk = "vt%d" % vi
                    for hg in range(HV // 512):
                        pi = self.rot("pv", 2)
                        for kc in range(KVC):
                            P.op("pe", lambda e, pi=pi, kc=kc, hg=hg, ts=ts: e.matmul(
                                pv[pi][:, :], ckn[:, kc, ts * 128:(ts + 1) * 128],
                                Wv[:, kc, hg * 4:(hg + 1) * 4, :].rearrange("p h d -> p (h d)"),
                                start=(kc == 0), stop=(kc == KVC - 1)), r=["Wv"] + ckk, w=["pv%d" % pi])
                        P.op("dve", lambda e, pi=pi, vi=vi, hg=hg: e.tensor_copy(
                            out=vt[vi][:, hg * 512:(hg + 1) * 512], in_=pv[pi][:, :]), r=["pv%d" % pi], w=[vk + "_%d" % hg])
                    r0 = t0 + ts * 128
                    P.dma("act", self.scr["vtok"].ap()[r0:r0 + 128, :], vt[vi][:, :],
                          r=[vk + "_%d" % hg for hg in range(HV // 512)], w=["vtok"])

    def phase_mla_attn(self, l):
        c, P = self.cfg, self.P
        H, NT, S, CTX = c.H, c.NT, c.S, c.CTX
        NKT = NT // 128
        scale = 192.0 ** -0.5
        with self.phase():
            kn = self.sb("kn", [128, NT], BF16)
            kr = self.sb("kr", [64, NT], BF16)
            vh = self.sb("vh", [128, NKT, 128], BF16)
            qn = self.sbn("qn", 2, [128, 512], BF16)
            qr = self.sbn("qr", 2, [64, 512], BF16)
            pt = self.sbn("pT", 3, [128, 512], BF16)
            rd = self.sb("rd", [128, 512])
            ob = self.sbn("ob", 2, [128, 512], BF16)
            pS = self.psn("pS", 2, [128, 512])
            pO = self.psn("pO", 2, [128, 512])
            pD = self.psn("pD", 2, [128, 512])
            P.dma("sp", kr[:], self.scr["krT"].ap(), r=["krT"], w=["kr"])
            for h in range(H):
                P.dma("sp", kn[:], self.scr["knT"].ap()[:, h, :], r=["knT"], w=["kn"])
                vsrc = self.scr["vtok"].ap()[:, h * 128:(h + 1) * 128].rearrange("(t p) d -> p t d", p=128)
                for k0_ in range(0, NKT, 8):
                    k1_ = min(NKT, k0_ + 8)
                    P.dma("sp", vh[:, k0_:k1_, :], vsrc[:, k0_:k1_, :], r=["vtok"], w=["vh"])
                for (t0, tw, v) in c.tiles(ctx=(l < c.L - 1)):
                    qi = self.rot("qn", 2)
                    qk = "q%d" % qi
                    P.dma("sp", qn[qi][:, :tw], self.scr["qnT"].ap()[:, h, t0:t0 + tw], r=["qnT"], w=[qk + "n"])
                    P.dma("sp", qr[qi][:, :tw], self.scr["qrT"].ap()[:, h, t0:t0 + tw], r=["qrT"], w=[qk + "r"])
                    kts = list(range(NKT)) if v == 0 else list(range(S // 128, NKT))
                    oi = self.rot("pO", 2)
                    for n_, kt in enumerate(kts):
                        si = self.rot("pS", 2)
                        P.op("pe", lambda e, si=si, kt=kt, qi=qi, tw=tw: e.matmul(
                            pS[si][:, :tw], kn[:, kt * 128:(kt + 1) * 128], qn[qi][:, :tw], start=True, stop=False),
                            r=["kn", qk + "n"], w=["pS%d" % si])
                        P.op("pe", lambda e, si=si, kt=kt, qi=qi, tw=tw: e.matmul(
                            pS[si][:, :tw], kr[:, kt * 128:(kt + 1) * 128], qr[qi][:, :tw], start=False, stop=True),
                            r=["kr", qk + "r"], w=["pS%d" % si])
                        pi = self.rot("pT", 3)
                        P.op("act", lambda e, si=si, pi=pi, tw=tw: e.activation(
                            out=pt[pi][:, :tw], in_=pS[si][:, :tw], func=AF.Exp, scale=scale), r=["pS%d" % si], w=["pT%d" % pi])
                        P.op("pe", lambda e, oi=oi, pi=pi, kt=kt, tw=tw, n_=n_, nk=len(kts): e.matmul(
                            pO[oi][:, :tw], vh[:, kt, :], pt[pi][:, :tw], start=(n_ == 0), stop=(n_ == nk - 1)),
                            r=["vh", "pT%d" % pi], w=["pO%d" % oi])
                        P.op("pe", lambda e, oi=oi, pi=pi, tw=tw, n_=n_, nk=len(kts): e.matmul(
                            pD[oi][:, :tw], self.ones_b[:, :], pt[pi][:, :tw], start=(n_ == 0), stop=(n_ == nk - 1)),
                            r=["ones_b", "pT%d" % pi], w=["pD%d" % oi])
                    bi = self.rot("ob", 2)
                    P.op("dve", lambda e, oi=oi, tw=tw: e.reciprocal(out=rd[:, :tw], in_=pD[oi][:, :tw]), r=["pD%d" % oi], w=["rd"])
                    P.op("dve", lambda e, oi=oi, bi=bi, tw=tw: e.tensor_tensor(
                        out=ob[bi][:, :tw], in0=pO[oi][:, :tw], in1=rd[:, :tw], op=ALU.mult), r=["pO%d" % oi, "rd"], w=["ob%d" % bi])
                    P.dma("act", self.scr["nT"].ap()[h * 128:(h + 1) * 128, t0:t0 + tw], ob[bi][:, :tw], r=["ob%d" % bi], w=["nT"])

    def mla_mixer(self, l):
        self.phase_norm(l, 0)
        hh = max(1, self.cfg.H // 2)
        for h0 in range(0, self.cfg.H, hh):
            self.phase_mla_q(l, h0, hh)
        self.phase_mla_kv(l)
        self.phase_mla_attn(l)
        self.phase_linear_res(l, "mla_w_o", None, ctx=(l < self.cfg.L - 1))

    def phase_diff_proj(self, col0, dst, blk0, nblk):
        c, P = self.cfg, self.P
        DC, NT = c.DC, c.NT
        with self.phase():
            W = self.sb("Wp", [128, DC, nblk, 128], BF16)
            Ws = self.sb("Wps", [128, DC, nblk, 128], BF16)
            nin = self.sbn("nin", 2, [128, DC, 512], BF16)
            rt = self.sbn("rt", 2, [128, 2, 512])
            ta = self.sbn("rta", 2, [128, 512])
            tb = self.sbn("rtb", 2, [128, 512])
            qa = self.sbn("qa", 2, [128, nblk, 512], BF16)
            p1 = self.psn("pr1", 2, [128, 512])
            p2 = self.psn("pr2", 2, [128, 512])
            wv = self.inp["diff_w_qkv"].ap().rearrange("(c p) o -> p c o", p=128)
            for b in range(nblk):
                cb = col0 + b * 128
                P.dma("pool", W[:, :, b, :], wv[:, :, cb:cb + 128], w=["Wp"])
                P.dma("pool", Ws[:, :, b, 0:64], wv[:, :, cb + 64:cb + 128], w=["Wps"])
                P.dma("pool", Ws[:, :, b, 64:128], wv[:, :, cb:cb + 64], w=["Wps"])
            nv = self.scr["nT"].ap().rearrange("(c p) n -> p c n", p=128)
            ropev = self.inp["rope_diff"].ap().rearrange("a p n -> p a n")
            for (t0, tw, v) in c.tiles():
                bi = self.rot("nin", 2)
                nk, rk, qk = "nin%d" % bi, "rt%d" % bi, "qa%d" % bi
                P.dma("sp", nin[bi][:, :, :tw], nv[:, :, t0:t0 + tw], r=["nT"], w=[nk])
                P.dma("sp", rt[bi][:, :, :tw], ropev[:, :, t0:t0 + tw], w=[rk])
                for b in range(nblk):
                    pi = self.rot("pr1", 2)
                    for cc in range(DC):
                        P.op("pe", lambda e, pi=pi, b=b, cc=cc, bi=bi, tw=tw: e.matmul(
                            p1[pi][:, :tw], W[:, cc, b, :], nin[bi][:, cc, :tw], start=(cc == 0), stop=(cc == DC - 1)),
                            r=["Wp", nk], w=["pr1%d" % pi])
                    for cc in range(DC):
                        P.op("pe", lambda e, pi=pi, b=b, cc=cc, bi=bi, tw=tw: e.matmul(
                            p2[pi][:, :tw], Ws[:, cc, b, :], nin[bi][:, cc, :tw], start=(cc == 0), stop=(cc == DC - 1)),
                            r=["Wps", nk], w=["pr2%d" % pi])
                    P.op("dve", lambda e, pi=pi, bi=bi, tw=tw: e.tensor_tensor(
                        out=ta[pi][:, :tw], in0=p1[pi][:, :tw], in1=rt[bi][:, 0, :tw], op=ALU.mult),
                        r=["pr1%d" % pi, rk], w=["rta%d" % pi])
                    P.op("dve", lambda e, pi=pi, bi=bi, tw=tw: e.tensor_tensor(
                        out=tb[pi][:, :tw], in0=p2[pi][:, :tw], in1=rt[bi][:, 1, :tw], op=ALU.mult),
                        r=["pr2%d" % pi, rk], w=["rtb%d" % pi])
                    P.op("pool", lambda e, pi=pi, bi=bi, b=b, tw=tw: e.tensor_tensor(
                        out=qa[bi][:, b, :tw], in0=ta[pi][:, :tw], in1=tb[pi][:, :tw], op=ALU.add),
                        r=["rta%d" % pi, "rtb%d" % pi], w=[qk + "_%d" % b])
                P.dma("act", self.scr[dst].ap()[:, blk0:blk0 + nblk, t0:t0 + tw], qa[bi][:, :, :tw],
                      r=[qk + "_%d" % b for b in range(nblk)], w=[dst])

    def phase_diff_v(self):
        c, P = self.cfg, self.P
        DC, D = c.DC, c.D
        with self.phase():
            Wv = self.sb("Wdv", [128, DC, D], BF16)
            nin = self.sbn("nin", 2, [128, DC, 512], BF16)
            vt = self.sbn("vt", 2, [128, D], BF16)
            pv = self.psn("pv", 4, [128, 512])
            wv = self.inp["diff_w_qkv"].ap().rearrange("(c p) o -> p c o", p=128)
            for c0 in range(0, DC, 4):
                P.dma("pool", Wv[:, c0:c0 + 4, :], wv[:, c0:c0 + 4, 2 * D:3 * D], w=["Wdv"])
            nv = self.scr["nT"].ap().rearrange("(c p) n -> p c n", p=128)
            for (t0, tw, v) in c.tiles():
                bi = self.rot("nin", 2)
                nk = "nin%d" % bi
                P.dma("sp", nin[bi][:, :, :tw], nv[:, :, t0:t0 + tw], r=["nT"], w=[nk])
                for ts in range(tw // 128):
                    vi = self.rot("vt", 2)
                    vk = "vt%d" % vi
                    for j in range(D // 512):
                        pi = self.rot("pv", 4)
                        for cc in range(DC):
                            P.op("pe", lambda e, pi=pi, cc=cc, j=j, ts=ts, bi=bi: e.matmul(
                                pv[pi][:, :], nin[bi][:, cc, ts * 128:(ts + 1) * 128], Wv[:, cc, j * 512:(j + 1) * 512],
                                start=(cc == 0), stop=(cc == DC - 1)), r=["Wdv", nk], w=["pv%d" % pi])
                        if j % 2 == 0:
                            P.op("dve", lambda e, pi=pi, vi=vi, j=j: e.tensor_copy(
                                out=vt[vi][:, j * 512:(j + 1) * 512], in_=pv[pi][:, :]), r=["pv%d" % pi], w=[vk + "_%d" % j])
                        else:
                            P.op("act", lambda e, pi=pi, vi=vi, j=j: e.activation(
                                out=vt[vi][:, j * 512:(j + 1) * 512], in_=pv[pi][:, :], func=AF.Copy),
                                r=["pv%d" % pi], w=[vk + "_%d" % j])
                    r0 = t0 + ts * 128
                    P.dma("act", self.scr["vtok"].ap()[r0:r0 + 128, :], vt[vi][:, :],
                          r=[vk + "_%d" % j for j in range(D // 512)], w=["vtok"])

    def phase_diff_attn(self, l):
        c, P = self.cfg, self.P
        HD, NT, S = c.HD, c.NT, c.S
        NKT = NT // 128
        scale = 128.0 ** -0.5
        lam_init = 0.8 - 0.6 * math.exp(-0.3 * l)
        with self.phase():
            lv = self.sb("lv", [128, 4, 128])
            pr = self.sb("lpr", [128, 2, 128])
            s12 = self.sb("s12", [128, 2])
            nlam = self.sb("nlam", [128, 1])
            sg = self.sb("sg", [128, 2])
            k0 = self.sb("k0", [128, NT], BF16)
            k1 = self.sb("k1", [128, NT], BF16)
            vh = self.sb("vh", [128, NKT, 256], BF16)
            q0 = self.sbn("q0", 2, [128, 512], BF16)
            q1 = self.sbn("q1", 2, [128, 512], BF16)
            pt = self.sbn("pT", 4, [128, 512], BF16)
            rd = self.sb("rd", [128, 2, 512])
            tt = self.sbn("tt", 2, [128, 512])
            aa = self.sb("aa", [128, 2, 512])
            sqa = self.sb("sqa", [128, 2, 512])
            rs = self.sb("rsd", [128, 512])
            ob = self.sbn("ob", 2, [128, 2, 512], BF16)
            pS = self.psn("pS", 2, [128, 512])
            pO = [self.psn("pO%d" % n, 2, [128, 512]) for n in range(2)]
            pD = self.psn("pD", 2, [128, 512])
            lamt = self.inp["diff_lam"]
            P.dma("sp", lv[:], bass.AP(lamt, 0, [[0, 128], [128, 4], [1, 128]]), w=["lv"])
            P.dma("sp", sg[:], self.inp["subln_gT"].ap(), w=["sg"])
            P.op("dve", lambda e: e.tensor_scalar(out=sg[:], in0=sg[:], scalar1=float(1.0 - lam_init), scalar2=None,
                                                  op0=ALU.mult), r=["sg"], w=["sg"])
            for i in range(2):
                P.op("dve", lambda e, i=i: e.tensor_tensor(out=pr[:, i, :], in0=lv[:, 2 * i, :], in1=lv[:, 2 * i + 1, :],
                                                           op=ALU.mult), r=["lv"], w=["lpr"])
                P.op("dve", lambda e, i=i: e.reduce_sum(out=s12[:, i:i + 1], in_=pr[:, i, :], axis=mybir.AxisListType.X),
                     r=["lpr"], w=["s12"])
            P.op("act", lambda e: e.activation(out=s12[:], in_=s12[:], func=AF.Exp), r=["s12"], w=["s12"])
            P.op("dve", lambda e: e.tensor_tensor(out=nlam[:], in0=s12[:, 1:2], in1=s12[:, 0:1], op=ALU.subtract),
                 r=["s12"], w=["nlam"])
            P.op("dve", lambda e: e.tensor_scalar(out=nlam[:], in0=nlam[:], scalar1=float(-lam_init), scalar2=None,
                                                  op0=ALU.add), r=["nlam"], w=["nlam"])
            ks = [k0, k1]
            for h in range(HD):
                for n in range(2):
                    P.dma("sp", ks[n][:], self.scr["knT"].ap()[:, 2 * h + n, :], r=["knT"], w=["k%d" % n])
                vsrc = self.scr["vtok"].ap()[:, h * 256:(h + 1) * 256].rearrange("(t p) d -> p t d", p=128)
                for k0_ in range(0, NKT, 8):
                    k1_ = min(NKT, k0_ + 8)
                    P.dma("sp", vh[:, k0_:k1_, :], vsrc[:, k0_:k1_, :], r=["vtok"], w=["vh"])
                for (t0, tw, v) in c.tiles(ctx=False):
                    qi = self.rot("q0", 2)
                    qs = [q0[qi], q1[qi]]
                    for n in range(2):
                        P.dma("sp", qs[n][:, :tw], self.scr["qnT"].ap()[:, 2 * h + n, t0:t0 + tw], r=["qnT"], w=["q%d_%d" % (n, qi)])
                    for kt in range(NKT):
                        for n in range(2):
                            P.op("pe", lambda e, n=n, kt=kt, qs=qs, tw=tw: e.matmul(
                                pS[n][:, :tw], ks[n][:, kt * 128:(kt + 1) * 128], qs[n][:, :tw], start=True, stop=True),
                                r=["k%d" % n, "q%d_%d" % (n, qi)], w=["pS%d" % n])
                            pi = self.rot("pT", 4)
                            P.op("act", lambda e, n=n, pi=pi, tw=tw: e.activation(
                                out=pt[pi][:, :tw], in_=pS[n][:, :tw], func=AF.Exp, scale=scale), r=["pS%d" % n], w=["pT%d" % pi])
                            for j in range(2):
                                P.op("pe", lambda e, n=n, j=j, pi=pi, kt=kt, tw=tw: e.matmul(
                                    pO[n][j][:, :tw], vh[:, kt, j * 128:(j + 1) * 128], pt[pi][:, :tw],
                                    start=(kt == 0), stop=(kt == NKT - 1)), r=["vh", "pT%d" % pi], w=["pO%d_%d" % (n, j)])
                            P.op("pe", lambda e, n=n, pi=pi, kt=kt, tw=tw: e.matmul(
                                pD[n][:, :tw], self.ones_b[:, :], pt[pi][:, :tw], start=(kt == 0), stop=(kt == NKT - 1)),
                                r=["ones_b", "pT%d" % pi], w=["pD%d" % n])
                    for n in range(2):
                        P.op("dve", lambda e, n=n, tw=tw: e.reciprocal(out=rd[:, n, :tw], in_=pD[n][:, :tw]),
                             r=["pD%d" % n], w=["rd%d" % n])
                    for j in range(2):
                        P.op("dve", lambda e, j=j, tw=tw: e.tensor_tensor(out=tt[0][:, :tw], in0=pO[0][j][:, :tw], in1=rd[:, 0, :tw],
                                                                         op=ALU.mult), r=["pO0_%d" % j, "rd0"], w=["tt0"])
                        P.op("dve", lambda e, j=j, tw=tw: e.tensor_tensor(out=tt[1][:, :tw], in0=pO[1][j][:, :tw], in1=rd[:, 1, :tw],
                                                                         op=ALU.mult), r=["pO1_%d" % j, "rd1"], w=["tt1"])
                        P.op("dve", lambda e, j=j, tw=tw: e.scalar_tensor_tensor(
                            out=aa[:, j, :tw], in0=tt[1][:, :tw], scalar=nlam[:, 0:1], in1=tt[0][:, :tw], op0=ALU.mult, op1=ALU.add),
                            r=["tt0", "tt1", "nlam"], w=["aa%d" % j])
                        P.op("act", lambda e, j=j, tw=tw: e.activation(out=sqa[:, j, :tw], in_=aa[:, j, :tw], func=AF.Square),
                             r=["aa%d" % j], w=["sqa%d" % j])
                    for j in range(2):
                        P.op("pe", lambda e, j=j, tw=tw: e.matmul(pS[0][:, :tw], self.ones_f[:, :], sqa[:, j, :tw],
                                                                  start=(j == 0), stop=(j == 1)), r=["sqa%d" % j, "ones_f"], w=["pS0"])
                    self.rsqrt(rs[:, :tw], pS[0][:, :tw], 1.0 / 256.0, r=["pS0"], w=["rsd"])
                    bi = self.rot("ob", 2)
                    for j in range(2):
                        P.op("dve", lambda e, j=j, tw=tw: e.tensor_tensor(out=aa[:, j, :tw], in0=aa[:, j, :tw], in1=rs[:, :tw],
                                                                         op=ALU.mult), r=["aa%d" % j, "rsd"], w=["aa%d" % j])
                        P.op("act", lambda e, j=j, bi=bi, tw=tw: e.activation(
                            out=ob[bi][:, j, :tw], in_=aa[:, j, :tw], func=AF.Identity, scale=sg[:, j:j + 1],
                            bias=self.zero_c[:, 0:1]), r=["aa%d" % j, "sg"], w=["ob%d_%d" % (bi, j)])
                    P.dma("act", self.scr["nT"].ap().rearrange("(c p) n -> p c n", p=128)[:, 2 * h:2 * h + 2, t0:t0 + tw],
                          ob[bi][:, :, :tw], r=["ob%d_%d" % (bi, j) for j in range(2)], w=["nT"])

    def diff_mixer(self, l):
        c = self.cfg
        self.phase_norm(l, 0)
        nb = 2 * c.HD
        half = max(1, nb // 2)
        for b0 in range(0, nb, half):
            self.phase_diff_proj(b0 * 128, "qnT", b0, half)
        for b0 in range(0, nb, half):
            self.phase_diff_proj(c.D + b0 * 128, "knT", b0, half)
        self.phase_diff_v()
        self.phase_diff_attn(l)
        self.phase_linear_res(l, "diff_w_o", None, ctx=False)

    def build(self, upto=99):
        self.declare()
        self.phase_consts()
        self.phase_copy_in()
        if upto == 99:
            for l in range(self.cfg.L):
                if l % 4 == 0:
                    self.conv_mixer(l)
                if l % 4 == 1:
                    self.mla_mixer(l)
                if l % 4 == 3:
                    self.diff_mixer(l)
                if l % 4 == 2:
                    self.phase_norm(l, 0)
                    self.phase_pool(l)
                self.moe(l)
        if upto == 1:
            self.phase_norm(0, 0)
        if upto == 2:
            self.moe(0)
        if upto == 8:
            self.diff_mixer(3)
        if upto == 7:
            self.mla_mixer(1)
        if upto == 6:
            self.conv_mixer(0)
        if upto == 5:
            self.phase_norm(2, 0)
            self.phase_pool(2)
        if upto == 4:
            self.phase_norm2_router(0)
            self.phase_tok()
        if upto == 3:
            self.phase_norm2_router(0)
            self.phase_tok()
            self.phase_topk(0)
            with self.phase():
                self.P.dma("sp", self.scr["dbg_idx"].ap(), self.idxT[:], w=["dbg"])
                self.P.dma("sp", self.scr["dbg_g"].ap(), self.gT[:], w=["dbg2"])
        self.phase_norm(0, 0, final=True)
        self.P.es.close()
        return self.nc


def host_inputs(cfg, b, x, c, ctx, c_ctx, ada_w, ada_b, norm_g, final_g, conv_pw1_w, conv_pw1_b, conv_dw_w, conv_dw_b,
                conv_ln_g, conv_ln_b, conv_pw2_w, conv_pw2_b, mla_w_dq, mla_q_norm, mla_w_uq, mla_w_dkv,
                mla_kv_norm, mla_w_ukv, mla_w_o, pool_w, pool_scale, diff_w_qkv, diff_lq1, diff_lk1, diff_lq2,
                diff_lk2, diff_subln_g, diff_w_o, moe_router, moe_w_gate, moe_w_up, moe_w_down):
    f = lambda a: np.ascontiguousarray(a, dtype=np.float32)
    D, NT, S, CTX, DC = cfg.D, cfg.NT, cfg.S, cfg.CTX, cfg.DC

    def fm(vec):
        return f(np.asarray(vec).reshape(-1, 128).T)

    m = {}
    m["xT_in"] = f(np.concatenate([np.asarray(x[b]).T, np.asarray(ctx[b]).T], axis=1))
    m["svec"] = f(np.stack([fm(c[b]), fm(c_ctx)], axis=-1))
    m["ada_w"] = f(ada_w)
    m["ada_bT"] = f(np.stack([fm(ada_b[l]) for l in range(cfg.L)], axis=1))
    m["normgT"] = f(np.stack([np.stack([fm(norm_g[l, k]) for k in range(2)], axis=1) for l in range(cfg.L)], axis=1))
    m["finalgT"] = fm(final_g)
    m["conv_pw1_w"] = f(conv_pw1_w); m["pw1_bT"] = fm(conv_pw1_b)
    m["dw_wT"] = f(np.asarray(conv_dw_w).T.reshape(DC, 128, cfg.CONVW).transpose(1, 0, 2))
    m["dw_bT"] = fm(conv_dw_b); m["ln_gT"] = fm(conv_ln_g); m["ln_bT"] = fm(conv_ln_b)
    m["conv_pw2_w"] = f(conv_pw2_w); m["pw2_bT"] = fm(conv_pw2_b)
    m["mla_w_dq"] = f(mla_w_dq); m["q_normT"] = fm(mla_q_norm); m["mla_w_uq"] = f(mla_w_uq)
    m["mla_w_dkv"] = f(mla_w_dkv); m["kv_normT"] = fm(mla_kv_norm); m["mla_w_ukv"] = f(mla_w_ukv)
    m["mla_w_o"] = f(mla_w_o)
    m["pool_w"] = f(pool_w); m["pool_scaleT"] = fm(pool_scale)
    m["diff_w_qkv"] = f(diff_w_qkv)
    m["diff_lam"] = f(np.concatenate([diff_lq1, diff_lk1, diff_lq2, diff_lk2])[None, :])
    m["subln_gT"] = fm(diff_subln_g); m["diff_w_o"] = f(diff_w_o)
    m["moe_router"] = f(moe_router); m["moe_w_gate"] = f(moe_w_gate); m["moe_w_up"] = f(moe_w_up)
    m["moe_w_down"] = f(moe_w_down)
    m["ident"] = np.eye(128, dtype=np.float32)
    m["padidx"] = f(np.tile((NT + np.arange(128, dtype=np.float32))[None, :], (cfg.E, 1)))
    rows = S // cfg.GRID_W

    def rope_tab(rot):
        q = rot // 4
        inv = (10000.0 ** (-np.arange(q, dtype=np.float32) / q)).astype(np.float32)
        ra = np.arange(rows, dtype=np.float32)[:, None, None] * inv
        ca = np.arange(cfg.GRID_W, dtype=np.float32)[None, :, None] * inv
        ang = np.concatenate([np.broadcast_to(ra, (rows, cfg.GRID_W, q)), np.broadcast_to(ca, (rows, cfg.GRID_W, q))], -1)
        ang = ang.reshape(S, 2 * q).astype(np.float32)
        cs, sn = np.cos(ang).T, np.sin(ang).T
        cosf = np.concatenate([np.concatenate([cs, cs], 0), np.ones((rot, CTX), np.float32)], 1)
        sinf = np.concatenate([np.concatenate([-sn, sn], 0), np.zeros((rot, CTX), np.float32)], 1)
        return f(np.stack([cosf, sinf]))
    m["rope_mla"] = rope_tab(64)
    m["rope_diff"] = rope_tab(128)
    inv = np.zeros((4, NT), np.float32)
    for g, w in enumerate((2, 4, 8, 16)):
        for (off, ln) in ((0, S), (S, CTX)):
            t = np.arange(ln)
            lo = np.clip(t - w // 2, 0, ln); hi = np.clip(t - w // 2 + w, 0, ln)
            inv[g, off:off + ln] = 1.0 / (hi - lo).astype(np.float32)
    m["pool_inv"] = inv
    return m


_CFG = Cfg()


def kernel(**inputs):
    cfg = _CFG
    bld = Builder(cfg)
    nc = bld.build()
    in_maps = [host_inputs(cfg, b, **inputs) for b in range(cfg.B)]
    res = run_bass_kernel_spmd(nc, in_maps, core_ids=list(range(cfg.B)))
    out = np.stack([np.ascontiguousarray(res.results[b]["outT"].T) for b in range(cfg.B)], axis=0)
    return out.astype(np.float32)
```

```python
import math
from contextlib import ExitStack
import numpy as np
import concourse.bass as bass
import concourse.mybir as mybir
from concourse.bass_utils import run_bass_kernel_spmd

F32 = mybir.dt.float32
BF16 = mybir.dt.bfloat16
U32 = mybir.dt.uint32
I32 = mybir.dt.int32
AF = mybir.ActivationFunctionType
ALU = mybir.AluOpType
EPS = 1e-6


class Cfg:
    def __init__(self, D=2048, S=16384, CTX=256, E=16, L=4, GRID_W=64, B=2):
        self.D, self.S, self.CTX, self.E, self.L, self.GRID_W, self.B = D, S, CTX, E, L, GRID_W, B
        self.NT = S + CTX
        self.DC = D // 128
        self.FF = D // 2
        self.FC = self.FF // 128
        self.H = D // 128
        self.QR = 3 * D // 8
        self.QC = self.QR // 128
        self.KVR = D // 4
        self.KVC = self.KVR // 128
        self.HD = D // 256
        self.PG = D // 4
        self.PGC = self.PG // 128
        self.CAP = max(1, 2 * S // E)
        self.CAPC = max(1, 2 * CTX // E)
        self.CONVW = 31

    def tiles(self, ctx=True):
        out = [(t0, 512, 0) for t0 in range(0, self.S, 512)]
        if ctx:
            for t0 in range(0, self.CTX, 512):
                out.append((self.S + t0, min(512, self.CTX - t0), 1))
        return out


class Prog:
    ENGS = ("pe", "act", "dve", "pool", "sp")
    NDSEM = 8

    def __init__(self, nc):
        self.nc = nc
        self.es = ExitStack()
        self.esem = {e: self.es.enter_context(nc.semaphore("s_" + e)) for e in self.ENGS}
        self.ecnt = {e: 0 for e in self.ENGS}
        self.dsem = {q: [self.es.enter_context(nc.semaphore("d_%s%d" % (q, i))) for i in range(self.NDSEM)]
                     for q in ("sp", "pool", "act")}
        self.dcnt = {q: [0] * self.NDSEM for q in self.dsem}
        self.drr = {q: 0 for q in self.dsem}
        self.waited = {e: {} for e in self.ENGS}
        self.ops = []
        self.state = {}
        self.nphase = 0

    def _rec(self, eng, fn, r, w, dma):
        deps = set()
        for k in r:
            st = self.state.get(k)
            if st and st[0] is not None:
                deps.add(st[0])
        for k in w:
            st = self.state.get(k)
            if st:
                if st[0] is not None:
                    deps.add(st[0])
                deps.update(st[1])
        oid = len(self.ops)
        self.ops.append(dict(eng=eng, fn=fn, deps=deps, dma=dma, ms=False))
        for k in r:
            st = self.state.setdefault(k, [None, []])
            st[1].append(oid)
        for k in w:
            self.state[k] = [oid, []]
        return oid

    def op(self, eng, fn, r=(), w=()):
        return self._rec(eng, fn, r, w, False)

    def dma(self, q, out, in_, r=(), w=(), **kw):
        return self._rec(q, lambda e: e.dma_start(out=out, in_=in_, **kw), r, w, True)

    def dma_fn(self, q, fn, r=(), w=()):
        return self._rec(q, fn, r, w, True)

    def flush(self):
        nc = self.nc
        ops = self.ops
        if not ops:
            return
        for o in ops:
            for d in o["deps"]:
                dd = ops[d]
                if dd["eng"] == "pe" and o["eng"] == "pe" and not dd["dma"] and not o["dma"]:
                    continue
                dd["ms"] = True
        last = {}
        for i, o in enumerate(ops):
            last[(o["eng"], o["dma"])] = i
        for (e, isd), i in last.items():
            if not isd:
                ops[i]["ms"] = True
        for o in ops:
            e = o["eng"]
            if o["dma"]:
                i = self.drr[e]
                self.drr[e] = (i + 1) % self.NDSEM
                o["sem"] = self.dsem[e][i]
                o["prev"] = self.dcnt[e][i]
                self.dcnt[e][i] += 16
                o["val"] = self.dcnt[e][i]
            elif o["ms"]:
                self.ecnt[e] += 1
                o["sem"] = self.esem[e]
                o["val"] = self.ecnt[e]
        per = {e: [] for e in self.ENGS}
        for o in ops:
            per[o["eng"]].append(o)
        targets = []
        for e in self.ENGS:
            if self.ecnt[e] > 0:
                targets.append((self.esem[e], self.ecnt[e]))
        for q in self.dsem:
            for i in range(self.NDSEM):
                if self.dcnt[q][i] > 0:
                    targets.append((self.dsem[q][i], self.dcnt[q][i]))
        waited = self.waited

        def emit_eng(ename):
            def run(eng):
                wd = waited[ename]

                def wait(sem, val):
                    key = sem.num if hasattr(sem, "num") else id(sem)
                    if wd.get(key, 0) < val:
                        eng.wait_ge(sem, val)
                        wd[key] = val

                for o in per[ename]:
                    if o["dma"] and o["prev"] > 0:
                        wait(o["sem"], o["prev"])
                    for d in sorted(o["deps"]):
                        dd = ops[d]
                        if dd["eng"] == "pe" and ename == "pe" and not dd["dma"] and not o["dma"]:
                            continue
                        wait(dd["sem"], dd["val"])
                    ins = o["fn"](eng)
                    if o["dma"]:
                        ins.then_inc(o["sem"], 16)
                    elif o["ms"]:
                        ins.then_inc(o["sem"], 1)
                for sem, val in targets:
                    wait(sem, val)
            return run

        with nc.Block() as block:
            block.sync(emit_eng("sp"))
            block.tensor(emit_eng("pe"))
            block.scalar(emit_eng("act"))
            block.vector(emit_eng("dve"))
            block.gpsimd(emit_eng("pool"))
        self.ops = []
        self.state = {}
        self.nphase += 1


def bcast_rows(ap_row, nparts):
    t = ap_row.tensor
    return bass.AP(t, ap_row.offset, [[0, nparts]] + [list(x) for x in ap_row.ap[-1:]])


class Builder:
    def __init__(self, cfg, debug=()):
        self.cfg = cfg
        self.debug = set(debug)
        self.nc = bass.Bass("TRN2", target_bir_lowering=False)
        self.P = Prog(self.nc)
        self.inp = {}
        self.scr = {}
        self.dbg_router = True
        self.dbg_tr = True

    def din(self, name, shape, dt=F32):
        t = self.nc.dram_tensor(name, list(shape), dt, kind="ExternalInput")
        self.inp[name] = t
        return t

    def dscr(self, name, shape, dt=F32):
        kind = "ExternalOutput" if name in self.debug else "Internal"
        t = self.nc.dram_tensor(name, list(shape), dt, kind=kind)
        self.scr[name] = t
        return t

    def declare(self):
        c = self.cfg
        D, NT, L, DC, E = c.D, c.NT, c.L, c.DC, c.E
        i = self.din
        i("xT_in", [D, NT])
        i("svec", [128, DC, 2])
        i("ada_w", [L, D, 6 * D])
        i("ada_bT", [128, L, 6 * DC])
        i("normgT", [128, L, 2, DC])
        i("finalgT", [128, DC])
        i("conv_pw1_w", [D, 2 * D]); i("pw1_bT", [128, 2 * DC]); i("dw_wT", [128, DC, c.CONVW])
        i("dw_bT", [128, DC]); i("ln_gT", [128, DC]); i("ln_bT", [128, DC])
        i("conv_pw2_w", [D, D]); i("pw2_bT", [128, DC])
        i("mla_w_dq", [D, c.QR]); i("q_normT", [128, c.QC]); i("mla_w_uq", [c.QR, c.H * 192])
        i("mla_w_dkv", [D, c.KVR + 64]); i("kv_normT", [128, c.KVC]); i("mla_w_ukv", [c.KVR, c.H * 256])
        i("mla_w_o", [D, D])
        i("pool_w", [4, c.PG, c.PG]); i("pool_scaleT", [128, DC]); i("pool_inv", [4, NT])
        i("diff_w_qkv", [D, 3 * D]); i("diff_lam", [1, 4 * 128]); i("subln_gT", [128, 2]); i("diff_w_o", [D, D])
        i("rope_mla", [2, 64, NT]); i("rope_diff", [2, 128, NT])
        i("moe_router", [L, D, E]); i("moe_w_gate", [L, E, D, c.FF]); i("moe_w_up", [L, E, D, c.FF])
        i("moe_w_down", [L, E, c.FF, D])
        self.outT = self.nc.dram_tensor("outT", [D, c.S], F32, kind="ExternalOutput")
        s = self.dscr
        s("xT", [D, NT])
        s("nT", [D, NT], BF16)
        s("macc", [NT + 128, D])
        s("mtok", [NT + 128, D], BF16)
        s("affT", [E, NT])
        s("uT", [D, NT])
        HH = max(c.H, 2 * c.HD)
        s("qnT", [128, HH, NT], BF16); s("qrT", [64, c.H, NT], BF16); s("knT", [128, HH, NT], BF16)
        s("krT", [64, NT], BF16); s("vtok", [NT, D], BF16)
        RT_ = (c.CAP + c.CAPC + 127) // 128
        s("dbg_idx", [128, RT_, E], I32)
        s("dbg_g", [128, RT_, E])
        i("ident", [128, 128])
        i("padidx", [E, 128])

    def phase(self):
        b = self

        class _Ph:
            def __enter__(s2):
                b._es = ExitStack()
                b._es.__enter__()
                b._rot = {}
                return s2

            def __exit__(s2, *a):
                if a[0] is None:
                    b.P.flush()
                b._es.__exit__(*a)
                return False
        return _Ph()

    def sb(self, name, shape, dt=F32):
        return self._es.enter_context(self.nc.sbuf_tensor("%s_p%d" % (name, self.P.nphase), list(shape), dt))

    def ps(self, name, shape, dt=F32):
        return self._es.enter_context(self.nc.psum_tensor("%s_p%d" % (name, self.P.nphase), list(shape), dt))

    def sbn(self, name, n, shape, dt=F32):
        return [self.sb("%s%d" % (name, i), shape, dt) for i in range(n)]

    def psn(self, name, n, shape, dt=F32):
        return [self.ps("%s%d" % (name, i), shape, dt) for i in range(n)]

    def rot(self, name, n):
        i = self._rot.get(name, 0)
        self._rot[name] = i + 1
        return i % n

    def phase_consts(self):
        c, P, nc = self.cfg, self.P, self.nc
        L, DC = c.L, c.DC
        NJ = 6 * DC
        A = nc.alloc_sbuf_tensor
        self.ones_f = A("ones_f", [128, 128], F32)
        self.ones_b = A("ones_b", [128, 128], BF16)
        self.id_f = A("id_f", [128, 128], F32)
        self.id_b = A("id_b", [128, 128], BF16)
        self.modv = A("modv", [128, L, 2, NJ], F32)
        self.modA = A("modA", [128, L, 2, 2, DC], F32)
        self.fing = A("fing", [128, DC], F32)
        self.zero_c = A("zero_c", [128, 1], F32)
        self.eps_c = A("eps_c", [128, 1], F32)
        RT_ = (c.CAP + c.CAPC + 127) // 128
        self.idxT = A("idxT", [128, RT_, c.E], I32)
        self.gT = A("gT", [128, RT_, c.E], F32)
        with self.phase():
            s_sb = self.sb("s_sb", [128, DC, 2])
            ab = self.sb("ab", [128, L, NJ])
            ng = self.sb("ng", [128, L, 2, DC])
            W = self.sbn("adaW", 2, [128, DC, 512])
            pm = self.ps("pm", [128, NJ, 2])
            I = self.inp
            P.op("dve", lambda e: e.memset(self.ones_f[:], 1.0), w=["ones_f"])
            P.op("dve", lambda e: e.memset(self.ones_b[:], 1.0), w=["ones_b"])
            P.op("dve", lambda e: e.memset(self.zero_c[:], 0.0), w=["zero_c"])
            P.op("dve", lambda e: e.memset(self.eps_c[:], EPS), w=["eps_c"])
            P.op("dve", lambda e: e.memset(self.idxT[:], 0), w=["idxT"])
            P.op("dve", lambda e: e.memset(self.gT[:], 0.0), w=["gT"])
            P.dma("sp", self.id_f[:], I["ident"].ap(), w=["id_f"])
            P.op("dve", lambda e: e.tensor_copy(out=self.id_b[:], in_=self.id_f[:]), r=["id_f"], w=["id_b"])
            P.dma("sp", self.fing[:], I["finalgT"].ap(), w=["fing"])
            P.dma("sp", s_sb[:], I["svec"].ap(), w=["s_sb"])
            P.dma("sp", ab[:], I["ada_bT"].ap(), w=["ab"])
            P.dma("sp", ng[:], I["normgT"].ap(), w=["ng"])
            P.op("act", lambda e: e.activation(out=s_sb[:], in_=s_sb[:], func=AF.Silu), r=["s_sb"], w=["s_sb"])
            npc = (6 * c.D) // 512
            for l in range(L):
                wv = I["ada_w"].ap()[l].rearrange("(c p) n -> p c n", p=128)
                for pc in range(npc):
                    bi = self.rot("adaW", 2)
                    wk = "adaW%d" % bi
                    P.dma("sp", W[bi][:], wv[:, :, pc * 512:(pc + 1) * 512], w=[wk])
                    for j4 in range(4):
                        j = pc * 4 + j4
                        for cc in range(DC):
                            P.op("pe", lambda e, bi=bi, j=j, j4=j4, cc=cc: e.matmul(
                                pm[:, j, :], W[bi][:, cc, j4 * 128:(j4 + 1) * 128], s_sb[:, cc, :],
                                start=(cc == 0), stop=(cc == DC - 1)), r=[wk, "s_sb"], w=["pm"])
                for v in range(2):
                    P.op("dve", lambda e, l=l, v=v: e.tensor_tensor(
                        out=self.modv[:, l, v, :], in0=pm[:, :, v], in1=ab[:, l, :], op=ALU.add),
                        r=["pm", "ab"], w=["modv"])
                    for k in range(2):
                        j0 = (1 + 3 * k) * DC
                        P.op("dve", lambda e, l=l, v=v, k=k, j0=j0: e.scalar_tensor_tensor(
                            out=self.modA[:, l, v, k, :], in0=self.modv[:, l, v, j0:j0 + DC], scalar=1.0,
                            in1=ng[:, l, k, :], op0=ALU.add, op1=ALU.mult), r=["modv", "ng"], w=["modA"])

    def rsqrt(self, out, in_, scale, r, w, eps=EPS):
        P = self.P
        P.op("act", lambda e: e.activation(out=out, in_=in_, func=AF.Sqrt, scale=scale, bias=self.eps_c[:, 0:1]),
             r=list(r) + ["eps_c"], w=w)
        P.op("dve", lambda e: e.reciprocal(out=out, in_=out), r=w, w=w)

    def modcol(self, l, v, j, cc):
        return self.modv[:, l, v, j * self.cfg.DC + cc: j * self.cfg.DC + cc + 1]

    def phase_copy_in(self):
        c, P = self.cfg, self.P
        with self.phase():
            buf = self.sbn("cpy", 2, [128, c.DC, 512])
            src = self.inp["xT_in"].ap().rearrange("(c p) n -> p c n", p=128)
            dst = self.scr["xT"].ap().rearrange("(c p) n -> p c n", p=128)
            for (t0, tw, v) in c.tiles():
                bi = self.rot("cpy", 2)
                k = "cpy%d" % bi
                P.dma("sp", buf[bi][:, :, :tw], src[:, :, t0:t0 + tw], w=[k])
                P.dma("act", dst[:, :, t0:t0 + tw], buf[bi][:, :, :tw], r=[k], w=["xT"])

    def phase_norm(self, l, k, final=False, src="xT"):
        c, P = self.cfg, self.P
        DC, D = c.DC, c.D
        with self.phase():
            xin = self.sbn("xin", 2, [128, DC, 512])
            sq = self.sb("sq", [128, DC, 512])
            tmp = sq
            rs = self.sbn("rs", 2, [128, 512])
            nb = self.sbn("nb", 2, [128, DC, 512], F32 if final else BF16)
            pp = self.psn("pn", 2, [128, 512])
            xv = self.scr[src].ap().rearrange("(c p) n -> p c n", p=128)
            if final:
                ov = self.outT.ap().rearrange("(c p) n -> p c n", p=128)
            else:
                ov = self.scr["nT"].ap().rearrange("(c p) n -> p c n", p=128)
            for (t0, tw, v) in c.tiles(ctx=not final):
                bi = self.rot("xin", 2)
                xk, rk, nk, pk = "xin%d" % bi, "rs%d" % bi, "nb%d" % bi, "pn%d" % bi
                P.dma("act" if final else "sp", xin[bi][:, :, :tw], xv[:, :, t0:t0 + tw], r=[src], w=[xk])
                P.op("act", lambda e, bi=bi, tw=tw: e.activation(out=sq[:, :, :tw], in_=xin[bi][:, :, :tw],
                                                                func=AF.Square), r=[xk], w=["sq"] + ["tmp%d" % q for q in range(DC)])
                for cc in range(DC):
                    P.op("pe", lambda e, bi=bi, tw=tw, cc=cc: e.matmul(
                        pp[bi][:, :tw], self.ones_f[:, :], sq[:, cc, :tw], start=(cc == 0), stop=(cc == DC - 1)),
                        r=["sq", "ones_f"], w=[pk])
                self.rsqrt(rs[bi][:, :tw], pp[bi][:, :tw], 1.0 / D, r=[pk], w=[rk])
                for cc in range(DC):
                    P.op("dve", lambda e, bi=bi, tw=tw, cc=cc: e.tensor_tensor(
                        out=tmp[:, cc, :tw], in0=xin[bi][:, cc, :tw], in1=rs[bi][:, :tw], op=ALU.mult),
                        r=[xk, rk, "sq"], w=["tmp%d" % cc])
                    if final:
                        P.op("act", lambda e, bi=bi, tw=tw, cc=cc: e.activation(
                            out=nb[bi][:, cc, :tw], in_=tmp[:, cc, :tw], func=AF.Identity,
                            scale=self.fing[:, cc:cc + 1], bias=self.zero_c[:, 0:1]),
                            r=["tmp%d" % cc, "fing"], w=[nk + "_%d" % cc])
                    else:
                        P.op("act", lambda e, bi=bi, tw=tw, cc=cc, v=v: e.activation(
                            out=nb[bi][:, cc, :tw], in_=tmp[:, cc, :tw], func=AF.Identity,
                            scale=self.modA[:, l, v, k, cc:cc + 1], bias=self.modcol(l, v, 3 * k, cc)),
                            r=["tmp%d" % cc, "modA", "modv"], w=[nk + "_%d" % cc])
                P.dma("act", ov[:, :, t0:t0 + tw], nb[bi][:, :, :tw],
                      r=[nk + "_%d" % cc for cc in range(DC)], w=["nT"])

    def phase_norm2_router(self, l):
        c, P = self.cfg, self.P
        DC, D, E = c.DC, c.D, c.E
        with self.phase():
            xin = self.sbn("xin", 2, [128, DC, 512])
            sq = self.sb("sq", [128, DC, 512])
            tmp = sq
            rs = self.sbn("rs", 2, [128, 512])
            mf = self.sb("mf", [128, DC, 512])
            mb = self.sb("mb", [128, DC, 512], BF16)
            rw = self.sb("rw", [128, DC, E])
            ex = self.sb("ex", [E, 512])
            af = self.sbn("af", 2, [E, 512])
            pp = self.psn("pn", 2, [128, 512])
            pr = self.ps("pr", [128, 512])
            p2 = self.ps("p2", [128, 512])
            xv = self.scr["xT"].ap().rearrange("(c p) n -> p c n", p=128)
            P.dma("sp", rw[:], self.inp["moe_router"].ap()[l].rearrange("(c p) e -> p c e", p=128), w=["rw"])
            for (t0, tw, v) in c.tiles():
                bi = self.rot("xin", 2)
                xk, rk, pk = "xin%d" % bi, "rs%d" % bi, "pn%d" % bi
                P.dma("sp", xin[bi][:, :, :tw], xv[:, :, t0:t0 + tw], r=["xT"], w=[xk])
                P.op("act", lambda e, bi=bi, tw=tw: e.activation(out=sq[:, :, :tw], in_=xin[bi][:, :, :tw],
                                                                func=AF.Square), r=[xk], w=["sq"] + ["tmp%d" % q for q in range(DC)])
                for cc in range(DC):
                    P.op("pe", lambda e, bi=bi, tw=tw, cc=cc: e.matmul(
                        pp[bi][:, :tw], self.ones_f[:, :], sq[:, cc, :tw], start=(cc == 0), stop=(cc == DC - 1)),
                        r=["sq", "ones_f"], w=[pk])
                self.rsqrt(rs[bi][:, :tw], pp[bi][:, :tw], 1.0 / D, r=[pk], w=[rk])
                for cc in range(DC):
                    P.op("dve", lambda e, bi=bi, tw=tw, cc=cc: e.tensor_tensor(
                        out=tmp[:, cc, :tw], in0=xin[bi][:, cc, :tw], in1=rs[bi][:, :tw], op=ALU.mult),
                        r=[xk, rk, "sq"], w=["tmp%d" % cc])
                    P.op("act", lambda e, tw=tw, cc=cc, v=v: e.activation(
                        out=mf[:, cc, :tw], in_=tmp[:, cc, :tw], func=AF.Identity,
                        scale=self.modA[:, l, v, 1, cc:cc + 1], bias=self.modcol(l, v, 3, cc)),
                        r=["tmp%d" % cc, "modA", "modv"], w=["mf%d" % cc])
                    P.op("pool", lambda e, tw=tw, cc=cc: e.tensor_copy(out=mb[:, cc, :tw], in_=mf[:, cc, :tw]),
                         r=["mf%d" % cc], w=["mb%d" % cc])
                for cc in range(DC if self.dbg_router else 0):
                    P.op("pe", lambda e, tw=tw, cc=cc: e.matmul(
                        pr[0:E, :tw], rw[:, cc, :], mf[:, cc, :tw], start=(cc == 0), stop=(cc == DC - 1)),
                        r=["rw", "mf%d" % cc], w=["pr"])
                if self.dbg_router:
                    P.op("act", lambda e, tw=tw: e.activation(out=ex[:, :tw], in_=pr[0:E, :tw], func=AF.Exp),
                         r=["pr"], w=["ex"])
                    P.op("pe", lambda e, tw=tw: e.matmul(p2[0:E, :tw], self.ones_f[0:E, 0:E], ex[:, :tw],
                                                         start=True, stop=True), r=["ex", "ones_f"], w=["p2"])
                    ai = self.rot("af", 2)
                    ak = "af%d" % ai
                    P.op("dve", lambda e, tw=tw, ai=ai: e.reciprocal(out=af[ai][:, :tw], in_=p2[0:E, :tw]),
                         r=["p2"], w=[ak])
                    P.op("dve", lambda e, tw=tw, ai=ai: e.tensor_tensor(out=af[ai][:, :tw], in0=ex[:, :tw],
                                                                       in1=af[ai][:, :tw], op=ALU.mult),
                         r=["ex", ak], w=[ak])
                    P.dma("act", self.scr["affT"].ap()[:, t0:t0 + tw], af[ai][:, :tw], r=[ak], w=["affT"])
                P.dma("sp", self.scr["nT"].ap().rearrange("(c p) n -> p c n", p=128)[:, :, t0:t0 + tw], mb[:, :, :tw],
                      r=["mb%d" % cc for cc in range(DC)], w=["nT"])

    def phase_tok(self):
        c, P = self.cfg, self.P
        DC, D = c.DC, c.D
        with self.phase():
            mb = self.sbn("mbt", 2, [128, DC, 512], BF16)
            mt = self.sbn("mt", 2, [128, D], BF16)
            nbk = max(1, D // 1024)
            pt = [self.psn("pt%d" % i, nbk, [128, 1024], BF16) for i in range(2)]
            nv = self.scr["nT"].ap().rearrange("(c p) n -> p c n", p=128)
            zb = self.sb("zb", [128, D], BF16)
            P.op("dve", lambda e: e.memset(zb[:], 0.0), w=["zb"])
            P.dma("sp", self.scr["mtok"].ap()[c.NT:c.NT + 128, :], zb[:], r=["zb"], w=["mtok_pad"])
            for (t0, tw, v) in c.tiles():
                bi = self.rot("mbt", 2)
                bk_ = "mbt%d" % bi
                P.dma("sp", mb[bi][:, :, :tw], nv[:, :, t0:t0 + tw], r=["nT"], w=[bk_])
                for ts in range(tw // 128):
                    mi = self.rot("mt", 2)
                    mk = "mt%d" % mi
                    for cc in range(DC):
                        bk, off = divmod(cc * 128, 1024)
                        P.op("pe", lambda e, mi=mi, cc=cc, ts=ts, bk=bk, off=off, bi=bi: e.transpose(
                            pt[mi][bk][:, off:off + 128], mb[bi][:, cc, ts * 128:(ts + 1) * 128], self.id_b[:, :]),
                            r=[bk_, "id_b"], w=["pt%d_%d" % (mi, bk)])
                    for bk in range(nbk):
                        wd = min(1024, D)
                        if bk % 2 == 0:
                            P.op("act", lambda e, mi=mi, bk=bk, wd=wd: e.activation(
                                out=mt[mi][:, bk * 1024:bk * 1024 + wd], in_=pt[mi][bk][:, :wd], func=AF.Copy),
                                r=["pt%d_%d" % (mi, bk)], w=[mk + "_%d" % bk])
                        else:
                            P.op("dve", lambda e, mi=mi, bk=bk, wd=wd: e.tensor_copy(
                                out=mt[mi][:, bk * 1024:bk * 1024 + wd], in_=pt[mi][bk][:, :wd]),
                                r=["pt%d_%d" % (mi, bk)], w=[mk + "_%d" % bk])
                    r0 = t0 + ts * 128
                    P.dma("act", self.scr["mtok"].ap()[r0:r0 + 128, :], mt[mi][:, :],
                          r=[mk + "_%d" % bk for bk in range(nbk)], w=["mtok"])

    def phase_topk(self, l):
        c, P = self.cfg, self.P
        E, S, CTX, CAP, CAPC = c.E, c.S, c.CTX, c.CAP, c.CAPC
        R = CAP + CAPC
        RT = (R + 127) // 128
        RP = RT * 128
        with self.phase():
            wl = self.sb("wl", [E, S])
            wc = self.sb("wc", [E, CTX])
            mx = self.sb("mx", [E, RP])
            ix = self.sb("ix", [E, RP], U32)
            ixf = self.sb("ixf", [E, RP])
            ptpa = self.ps("ptpa", [128, 512])
            ptpb = self.ps("ptpb", [128, 512])
            P.op("dve", lambda e: e.memset(mx[:], 0.0), w=["mx"])
            P.op("dve", lambda e: e.memset(ix[:], 0), w=["ix"])
            P.dma("sp", wl[:], self.scr["affT"].ap()[:, 0:S], r=["affT"], w=["wl"])
            P.dma("sp", wc[:], self.scr["affT"].ap()[:, S:S + CTX], r=["affT"], w=["wc"])
            for (wk, work, n, base) in (("wl", wl, CAP, 0), ("wc", wc, CAPC, CAP)):
                for r8 in range(n // 8):
                    o = base + r8 * 8
                    P.op("dve", lambda e, work=work, o=o: e.max(out=mx[:, o:o + 8], in_=work[:]), r=[wk], w=["mx"])
                    P.op("dve", lambda e, work=work, o=o: e.max_index(ix[:, o:o + 8], mx[:, o:o + 8], work[:]),
                         r=[wk, "mx"], w=["ix"])
                    P.op("dve", lambda e, work=work, o=o: e.match_replace(
                        out=work[:], in_to_replace=mx[:, o:o + 8], in_values=work[:], imm_value=-1.0),
                        r=["mx"], w=[wk])
            P.op("dve", lambda e: e.tensor_copy(out=ixf[:], in_=ix[:]), r=["ix"], w=["ixf"])
            P.op("dve", lambda e: e.tensor_scalar(out=ixf[:, CAP:R], in0=ixf[:, CAP:R], scalar1=float(S), scalar2=None,
                                                  op0=ALU.add), r=["ixf"], w=["ixf"])
            if RP > R:
                P.dma("sp", ixf[:, R:RP], self.inp["padidx"].ap()[:, R - (RT - 1) * 128:128], r=["ixf"], w=["ixf"])
            for rt in range(RT):
                P.op("pe", lambda e, rt=rt: e.transpose(
                    ptpa[:, 0:E], ixf[:, rt * 128:(rt + 1) * 128], self.id_f[0:E, 0:E]),
                    r=["ixf", "id_f"], w=["ptpa"])
                P.op("pe", lambda e, rt=rt: e.transpose(
                    ptpb[:, 0:E], mx[:, rt * 128:(rt + 1) * 128], self.id_f[0:E, 0:E]),
                    r=["mx", "id_f"], w=["ptpb"])
                P.op("dve", lambda e, rt=rt: e.tensor_copy(out=self.idxT[:, rt, :], in_=ptpa[:, 0:E]),
                     r=["ptpa"], w=["idxT"])
                P.op("dve", lambda e, rt=rt: e.tensor_copy(out=self.gT[:, rt, :], in_=ptpb[:, 0:E]),
                     r=["ptpb"], w=["gT"])

    def phase_experts(self, l):
        c, P, nc = self.cfg, self.P, self.nc
        E, D, DC, FF, FC, CAP, CAPC = c.E, c.D, c.DC, c.FF, c.FC, c.CAP, c.CAPC
        R = CAP + CAPC
        RT = (R + 127) // 128
        with self.phase():
            zt = self.sb("zt", [128, D])
            Wg = self.sb("Wg", [128, DC, FF], BF16)
            Wu = self.sb("Wu", [128, DC, FF], BF16)
            Wd = self.sb("Wd", [128, FC, D], BF16)
            xg = self.sbn("xg", 2, [128, D], BF16)
            xsT = self.sb("xsT", [128, DC, 512], BF16)
            hT = self.sb("hT", [128, FC, 512], BF16)
            sg = self.sbn("sg", 2, [128, 512])
            yb = self.sbn("yb", 2, [128, D])
            ptr = self.psn("ptr", 2, [128, 1024], BF16)
            pg = self.psn("pg", 2, [128, 512])
            pu = self.psn("pu", 2, [128, 512])
            py = self.psn("py", 2, [128, 512])
            macc = self.scr["macc"].ap()
            mtok = self.scr["mtok"].ap()
            P.op("dve", lambda e: e.memset(zt[:], 0.0), w=["zt"])
            for r0 in range(0, c.NT + 128, 128):
                P.dma("sp", macc[r0:r0 + 128, :], zt[:], r=["zt"], w=["macc"])
            groups = [list(range(g, min(g + 4, RT))) for g in range(0, RT, 4)]
            for e_ in range(E):
                P.dma("pool", Wg[:], self.inp["moe_w_gate"].ap()[l, e_].rearrange("(c p) f -> p c f", p=128), w=["Wg"])
                P.dma("pool", Wu[:], self.inp["moe_w_up"].ap()[l, e_].rearrange("(c p) f -> p c f", p=128), w=["Wu"])
                P.dma("pool", Wd[:], self.inp["moe_w_down"].ap()[l, e_].rearrange("(c p) d -> p c d", p=128), w=["Wd"])
                for grp in groups:
                    gw = 0
                    offs = []
                    for rt in grp:
                        rows = 128
                        offs.append((rt, gw, rows))
                        gw += rows
                    for (rt, go, rows) in offs:
                        xi = self.rot("xg", 2)
                        xk = "xg%d" % xi
                        P.dma_fn("pool", lambda e, xi=xi, rt=rt, rows=rows, e_=e_: e.indirect_dma_start(
                            out=xg[xi][:rows, :], out_offset=None, in_=mtok,
                            in_offset=bass.IndirectOffsetOnAxis(ap=self.idxT[:rows, rt, e_:e_ + 1], axis=0)),
                            r=["mtok", "idxT"], w=[xk])
                        for c0 in range(0, DC, 8):
                            ti = self.rot("ptr", 2)
                            n8 = min(8, DC - c0)
                            for ci in range(n8):
                                P.op("pe", lambda e, ti=ti, ci=ci, c0=c0, xi=xi, rows=rows: e.transpose(
                                    ptr[ti][:, ci * 128:ci * 128 + rows],
                                    xg[xi][:rows, (c0 + ci) * 128:(c0 + ci + 1) * 128], self.id_b[:rows, :rows]),
                                    r=[xk, "id_b"], w=["ptr%d" % ti])
                            P.op("dve", lambda e, ti=ti, c0=c0, n8=n8, go=go, rows=rows: e.tensor_copy(
                                out=xsT[:, c0:c0 + n8, go:go + rows],
                                in_=ptr[ti][:, 0:n8 * 128].rearrange("p (c r) -> p c r", r=128)[:, :, :rows]),
                                r=["ptr%d" % ti], w=["xsT"])
                    for fc in range(FC):
                        gi = self.rot("pg", 2)
                        for cc in range(DC):
                            P.op("pe", lambda e, gi=gi, fc=fc, cc=cc, gw=gw: e.matmul(
                                pg[gi][:, :gw], Wg[:, cc, fc * 128:(fc + 1) * 128], xsT[:, cc, :gw],
                                start=(cc == 0), stop=(cc == DC - 1)), r=["Wg", "xsT"], w=["pg%d" % gi])
                        for cc in range(DC):
                            P.op("pe", lambda e, gi=gi, fc=fc, cc=cc, gw=gw: e.matmul(
                                pu[gi][:, :gw], Wu[:, cc, fc * 128:(fc + 1) * 128], xsT[:, cc, :gw],
                                start=(cc == 0), stop=(cc == DC - 1)), r=["Wu", "xsT"], w=["pu%d" % gi])
                        P.op("act", lambda e, gi=gi, gw=gw: e.activation(out=sg[gi][:, :gw], in_=pg[gi][:, :gw],
                                                                        func=AF.Silu), r=["pg%d" % gi], w=["sg%d" % gi])
                        P.op("dve", lambda e, gi=gi, gw=gw, fc=fc: e.tensor_tensor(
                            out=hT[:, fc, :gw], in0=sg[gi][:, :gw], in1=pu[gi][:, :gw], op=ALU.mult),
                            r=["sg%d" % gi, "pu%d" % gi], w=["hT%d" % fc])
                    for (rt, go, rows) in offs:
                        yi = self.rot("yb", 2)
                        yk = "yb%d" % yi
                        for j in range(D // 512):
                            pi = self.rot("py", 2)
                            for fc in range(FC):
                                P.op("pe", lambda e, pi=pi, fc=fc, go=go, rows=rows, j=j: e.matmul(
                                    py[pi][:rows, :], hT[:, fc, go:go + rows], Wd[:, fc, j * 512:(j + 1) * 512],
                                    start=(fc == 0), stop=(fc == FC - 1)), r=["hT%d" % fc, "Wd"], w=["py%d" % pi])
                            P.op("act", lambda e, pi=pi, yi=yi, rows=rows, j=j, rt=rt, e_=e_: e.activation(
                                out=yb[yi][:rows, j * 512:(j + 1) * 512], in_=py[pi][:rows, :], func=AF.Copy,
                                scale=self.gT[:rows, rt, e_:e_ + 1]), r=["py%d" % pi, "gT"], w=[yk + "_%d" % j])
                        P.dma_fn("pool", lambda e, yi=yi, rt=rt, rows=rows, e_=e_: e.indirect_dma_start(
                            out=macc, out_offset=bass.IndirectOffsetOnAxis(ap=self.idxT[:rows, rt, e_:e_ + 1], axis=0),
                            in_=yb[yi][:rows, :], in_offset=None, compute_op=ALU.add),
                            r=[yk + "_%d" % j for j in range(D // 512)] + ["idxT"], w=["macc"])

    def phase_combine(self, l):
        c, P = self.cfg, self.P
        DC, D = c.DC, c.D
        with self.phase():
            xin = self.sbn("xin", 2, [128, DC, 512])
            mc = self.sbn("mc", 2, [128, 4, D])
            pc = self.psn("pc", 4, [128, 512])
            xv = self.scr["xT"].ap().rearrange("(c p) n -> p c n", p=128)
            macc = self.scr["macc"].ap()
            for (t0, tw, v) in c.tiles():
                bi = self.rot("xin", 2)
                xk, mk = "xin%d" % bi, "mc%d" % bi
                ns = tw // 128
                P.dma("sp", xin[bi][:, :, :tw], xv[:, :, t0:t0 + tw], r=["xT"], w=[xk])
                P.dma("sp", mc[bi][:, :ns, :], macc[t0:t0 + tw, :].rearrange("(s p) d -> p s d", p=128),
                      r=["macc"], w=[mk])
                for cc in range(DC):
                    pi = self.rot("pc", 4)
                    for s_ in range(ns):
                        P.op("pe", lambda e, pi=pi, s_=s_, cc=cc, bi=bi: e.transpose(
                            pc[pi][:, s_ * 128:(s_ + 1) * 128], mc[bi][:, s_, cc * 128:(cc + 1) * 128], self.id_f[:, :]),
                            r=[mk, "id_f"], w=["pc%d" % pi])
                    P.op("dve", lambda e, pi=pi, cc=cc, bi=bi, tw=tw, v=v: e.scalar_tensor_tensor(
                        out=xin[bi][:, cc, :tw], in0=pc[pi][:, :tw], scalar=self.modcol(l, v, 5, cc),
                        in1=xin[bi][:, cc, :tw], op0=ALU.mult, op1=ALU.add), r=["pc%d" % pi, xk, "modv"], w=[xk])
                P.dma("act", xv[:, :, t0:t0 + tw], xin[bi][:, :, :tw], r=[xk], w=["xT"])

    def moe(self, l):
        self.phase_norm2_router(l)
        self.phase_tok()
        self.phase_topk(l)
        self.phase_experts(l)
        self.phase_combine(l)

    def phase_pool(self, l):
        c, P = self.cfg, self.P
        DC, D, PGC, PG, NT, S = c.DC, c.D, c.PGC, c.PG, c.NT, c.S
        HW = 8
        with self.phase():
            pw = self.sb("pw", [128, 4, PGC, PG], BF16)
            psc = self.sb("psc", [128, DC])
            sc = self.sb("sc", [128, 2, DC])
            nw = self.sbn("nw", 2, [128, DC, 512 + 2 * HW], BF16)
            invb = self.sbn("invb", 2, [128, 4, 512])
            ta = self.sbn("ta", 2, [128, 512 + 2 * HW])
            tb = self.sbn("tb", 2, [128, 512 + 2 * HW])
            dT = self.sbn("dT", 2, [128, DC, 512], BF16)
            xin = self.sbn("xin", 2, [128, DC, 512])
            pp = self.psn("ppool", 4, [128, 512])
            for g in range(4):
                P.dma("pool", pw[:, g], self.inp["pool_w"].ap()[g].rearrange("(k p) o -> p k o", p=128), w=["pw"])
            P.dma("sp", psc[:], self.inp["pool_scaleT"].ap(), w=["psc"])
            for v in range(2):
                P.op("dve", lambda e, v=v: e.tensor_tensor(out=sc[:, v, :], in0=psc[:], in1=self.modv[:, l, v, 2 * DC:3 * DC],
                                                          op=ALU.mult), r=["psc", "modv"], w=["sc"])
            nv = self.scr["nT"].ap().rearrange("(c p) n -> p c n", p=128)
            xv = self.scr["xT"].ap().rearrange("(c p) n -> p c n", p=128)
            invt = self.inp["pool_inv"]
            for (t0, tw, v) in c.tiles():
                lo_seq, hi_seq = (0, S) if v == 0 else (S, NT)
                bi = self.rot("nw", 2)
                nk, ik, dk, xk = "nw%d" % bi, "invb%d" % bi, "dT%d" % bi, "xin%d" % bi
                W = tw + 2 * HW
                a0, a1 = max(t0 - HW, lo_seq), min(t0 + tw + HW, hi_seq)
                P.op("pool", lambda e, bi=bi: e.memset(nw[bi][:], 0.0), w=[nk])
                P.dma("sp", nw[bi][:, :, a0 - (t0 - HW):a1 - (t0 - HW)], nv[:, :, a0:a1], r=["nT"], w=[nk])
                P.dma("sp", invb[bi][:, :, :tw], bass.AP(invt, t0, [[0, 128], [NT, 4], [1, tw]]), w=[ik])
                P.dma("sp", xin[bi][:, :, :tw], xv[:, :, t0:t0 + tw], r=["xT"], w=[xk])
                for cc in range(DC):
                    g = cc // PGC
                    ti = self.rot("ta", 2)
                    A, Bf = ta[ti], tb[ti]
                    ak, bk = "ta%d" % ti, "tb%d" % ti
                    src = nw[bi][:, cc, :]
                    P.op("dve", lambda e, A=A, src=src, W=W: e.tensor_tensor(out=A[:, 1:W], in0=src[:, 0:W - 1], in1=src[:, 1:W],
                                                                           op=ALU.add), r=[nk], w=[ak])
                    cur, curk, oth, othk = A, ak, Bf, bk
                    lo, hi, sh = 1, W, 1
                    for step in range(g):
                        nlo, nhi = lo + sh, hi - sh
                        P.op("dve", lambda e, cur=cur, oth=oth, nlo=nlo, nhi=nhi, sh=sh: e.tensor_tensor(
                            out=oth[:, nlo:nhi], in0=cur[:, nlo - sh:nhi - sh], in1=cur[:, nlo + sh:nhi + sh], op=ALU.add),
                            r=[curk], w=[othk])
                        cur, curk, oth, othk = oth, othk, cur, curk
                        lo, hi, sh = nlo, nhi, sh * 2
                    P.op("dve", lambda e, cur=cur, oth=oth, bi=bi, g=g, tw=tw: e.tensor_tensor(
                        out=oth[:, HW:HW + tw], in0=cur[:, HW:HW + tw], in1=invb[bi][:, g, :tw], op=ALU.mult),
                        r=[curk, ik], w=[othk])
                    P.op("dve", lambda e, oth=oth, bi=bi, cc=cc, tw=tw, src=src: e.tensor_tensor(
                        out=dT[bi][:, cc, :tw], in0=oth[:, HW:HW + tw], in1=src[:, HW:HW + tw], op=ALU.subtract),
                        r=[othk, nk], w=[dk + "_%d" % cc])
                for f in range(DC):
                    g, fl = divmod(f, PGC)
                    pi = self.rot("ppool", 4)
                    for kc in range(PGC):
                        P.op("pe", lambda e, pi=pi, g=g, kc=kc, fl=fl, bi=bi, tw=tw: e.matmul(
                            pp[pi][:, :tw], pw[:, g, kc, fl * 128:(fl + 1) * 128], dT[bi][:, g * PGC + kc, :tw],
                            start=(kc == 0), stop=(kc == PGC - 1)), r=["pw", dk + "_%d" % (g * PGC + kc)], w=["ppool%d" % pi])
                    P.op("dve", lambda e, pi=pi, f=f, bi=bi, tw=tw, v=v: e.scalar_tensor_tensor(
                        out=xin[bi][:, f, :tw], in0=pp[pi][:, :tw], scalar=sc[:, v, f:f + 1], in1=xin[bi][:, f, :tw],
                        op0=ALU.mult, op1=ALU.add), r=["ppool%d" % pi, "sc", xk], w=[xk])
                P.dma("act", xv[:, :, t0:t0 + tw], xin[bi][:, :, :tw], r=[xk], w=["xT"])

    def phase_linear_res(self, l, wname, bias_name=None, src="nT", ctx=True, wsel=None):
        c, P = self.cfg, self.P
        DC, D = c.DC, c.D
        with self.phase():
            W = self.sb("Wl", [128, DC, D], BF16)
            hin = self.sbn("hin", 2, [128, DC, 512], BF16)
            xin = self.sbn("xin", 2, [128, DC, 512])
            tb = self.sbn("lt", 2, [128, 512])
            pp = self.psn("pl", 4, [128, 512])
            wap = self.inp[wname].ap()
            for c0 in range(0, DC, 4):
                P.dma("pool", W[:, c0:c0 + 4, :], wap.rearrange("(c p) o -> p c o", p=128)[:, c0:c0 + 4, :], w=["Wl"])
            if bias_name:
                bs = self.sb("bs", [128, DC])
                P.dma("sp", bs[:], self.inp[bias_name].ap(), w=["bs"])
            hv = self.scr[src].ap().rearrange("(c p) n -> p c n", p=128)
            xv = self.scr["xT"].ap().rearrange("(c p) n -> p c n", p=128)
            for (t0, tw, v) in c.tiles(ctx=ctx):
                bi = self.rot("hin", 2)
                hk, xk = "hin%d" % bi, "xin%d" % bi
                P.dma("sp", hin[bi][:, :, :tw], hv[:, :, t0:t0 + tw], r=[src], w=[hk])
                P.dma("sp", xin[bi][:, :, :tw], xv[:, :, t0:t0 + tw], r=["xT"], w=[xk])
                for f in range(DC):
                    pi = self.rot("pl", 4)
                    for cc in range(DC):
                        P.op("pe", lambda e, pi=pi, f=f, cc=cc, bi=bi, tw=tw: e.matmul(
                            pp[pi][:, :tw], W[:, cc, f * 128:(f + 1) * 128], hin[bi][:, cc, :tw],
                            start=(cc == 0), stop=(cc == DC - 1)), r=["Wl", hk], w=["pl%d" % pi])
                    if bias_name:
                        ti = self.rot("lt", 2)
                        P.op("act", lambda e, pi=pi, ti=ti, f=f, tw=tw: e.activation(
                            out=tb[ti][:, :tw], in_=pp[pi][:, :tw], func=AF.Identity, scale=1.0, bias=bs[:, f:f + 1]),
                            r=["pl%d" % pi, "bs"], w=["lt%d" % ti])
                        P.op("dve", lambda e, ti=ti, f=f, bi=bi, tw=tw, v=v: e.scalar_tensor_tensor(
                            out=xin[bi][:, f, :tw], in0=tb[ti][:, :tw], scalar=self.modcol(l, v, 2, f),
                            in1=xin[bi][:, f, :tw], op0=ALU.mult, op1=ALU.add), r=["lt%d" % ti, "modv", xk], w=[xk])
                    else:
                        P.op("dve", lambda e, pi=pi, f=f, bi=bi, tw=tw, v=v: e.scalar_tensor_tensor(
                            out=xin[bi][:, f, :tw], in0=pp[pi][:, :tw], scalar=self.modcol(l, v, 2, f),
                            in1=xin[bi][:, f, :tw], op0=ALU.mult, op1=ALU.add), r=["pl%d" % pi, "modv", xk], w=[xk])
                P.dma("act", xv[:, :, t0:t0 + tw], xin[bi][:, :, :tw], r=[xk], w=["xT"])

    def phase_conv1(self, l):
        c, P = self.cfg, self.P
        DC, D = c.DC, c.D
        HC = DC // 2
        with self.phase():
            Wa = self.sb("Wa", [128, DC, HC * 128], BF16)
            Wg = self.sb("Wgt", [128, DC, HC * 128], BF16)
            b1 = self.sb("b1", [128, 2 * DC])
            nin = self.sbn("nin", 2, [128, DC, 512], BF16)
            sgm = self.sbn("sgm", 2, [128, 512])
            ub = self.sbn("ub", 2, [128, HC, 512])
            pa = self.psn("pa", 2, [128, 512])
            pg = self.psn("pgt", 2, [128, 512])
            wv = self.inp["conv_pw1_w"].ap().rearrange("(c p) o -> p c o", p=128)
            nv = self.scr["nT"].ap().rearrange("(c p) n -> p c n", p=128)
            uv = self.scr["uT"].ap().rearrange("(c p) n -> p c n", p=128)
            P.dma("sp", b1[:], self.inp["pw1_bT"].ap(), w=["b1"])
            for half in range(2):
                f0 = half * HC
                for c0 in range(0, DC, 4):
                    P.dma("pool", Wa[:, c0:c0 + 4, :], wv[:, c0:c0 + 4, f0 * 128:(f0 + HC) * 128], w=["Wa"])
                    P.dma("pool", Wg[:, c0:c0 + 4, :], wv[:, c0:c0 + 4, D + f0 * 128:D + (f0 + HC) * 128], w=["Wgt"])
                for (t0, tw, v) in c.tiles():
                    bi = self.rot("nin", 2)
                    nk, uk = "nin%d" % bi, "ub%d" % bi
                    P.dma("sp", nin[bi][:, :, :tw], nv[:, :, t0:t0 + tw], r=["nT"], w=[nk])
                    for fl in range(HC):
                        f = f0 + fl
                        pi = self.rot("pa", 2)
                        for cc in range(DC):
                            P.op("pe", lambda e, pi=pi, fl=fl, cc=cc, bi=bi, tw=tw: e.matmul(
                                pa[pi][:, :tw], Wa[:, cc, fl * 128:(fl + 1) * 128], nin[bi][:, cc, :tw],
                                start=(cc == 0), stop=(cc == DC - 1)), r=["Wa", nk], w=["pa%d" % pi])
                        for cc in range(DC):
                            P.op("pe", lambda e, pi=pi, fl=fl, cc=cc, bi=bi, tw=tw: e.matmul(
                                pg[pi][:, :tw], Wg[:, cc, fl * 128:(fl + 1) * 128], nin[bi][:, cc, :tw],
                                start=(cc == 0), stop=(cc == DC - 1)), r=["Wgt", nk], w=["pgt%d" % pi])
                        P.op("act", lambda e, pi=pi, f=f, tw=tw: e.activation(
                            out=sgm[pi][:, :tw], in_=pg[pi][:, :tw], func=AF.Sigmoid, scale=1.0,
                            bias=b1[:, DC + f:DC + f + 1]), r=["pgt%d" % pi, "b1"], w=["sgm%d" % pi])
                        P.op("dve", lambda e, pi=pi, f=f, fl=fl, bi=bi, tw=tw: e.scalar_tensor_tensor(
                            out=ub[bi][:, fl, :tw], in0=pa[pi][:, :tw], scalar=b1[:, f:f + 1], in1=sgm[pi][:, :tw],
                            op0=ALU.add, op1=ALU.mult), r=["pa%d" % pi, "sgm%d" % pi, "b1"], w=[uk + "_%d" % fl])
                    P.dma("act", uv[:, f0:f0 + HC, t0:t0 + tw], ub[bi][:, :, :tw],
                          r=[uk + "_%d" % fl for fl in range(HC)], w=["uT"])

    def phase_conv2(self, l):
        c, P = self.cfg, self.P
        DC, D, NT, S, KW = c.DC, c.D, c.NT, c.S, c.CONVW
        HW = KW // 2
        with self.phase():
            dw = self.sb("dw", [128, DC, KW])
            dwb = self.sb("dwb", [128, DC])
            lg = self.sb("lg", [128, DC])
            lb = self.sb("lb", [128, DC])
            uw = self.sbn("uw", 2, [128, DC, 512 + 2 * HW])
            vv = self.sb("vv", [128, DC, 512])
            sqt = self.sbn("sqt", 2, [128, 512])
            mu = self.sb("mu", [128, 512])
            msq = self.sb("msq", [128, 512])
            rstd = self.sb("rstd", [128, 512])
            t1 = self.sbn("t1", 2, [128, 512])
            hb = self.sbn("hb", 2, [128, DC, 512], BF16)
            pm = self.ps("pmn", [128, 512])
            p2 = self.ps("pm2", [128, 512])
            I = self.inp
            P.dma("sp", dw[:], I["dw_wT"].ap(), w=["dw"])
            P.dma("sp", dwb[:], I["dw_bT"].ap(), w=["dwb"])
            P.dma("sp", lg[:], I["ln_gT"].ap(), w=["lg"])
            P.dma("sp", lb[:], I["ln_bT"].ap(), w=["lb"])
            uv = self.scr["uT"].ap().rearrange("(c p) n -> p c n", p=128)
            nv = self.scr["nT"].ap().rearrange("(c p) n -> p c n", p=128)
            for (t0, tw, v) in c.tiles():
                lo_seq, hi_seq = (0, S) if v == 0 else (S, NT)
                bi = self.rot("uw", 2)
                uk, hk = "uw%d" % bi, "hb%d" % bi
                a0, a1 = max(t0 - HW, lo_seq), min(t0 + tw + HW, hi_seq)
                if a0 > t0 - HW or a1 < t0 + tw + HW:
                    P.op("pool", lambda e, bi=bi: e.memset(uw[bi][:], 0.0), w=[uk])
                P.dma("sp", uw[bi][:, :, a0 - (t0 - HW):a1 - (t0 - HW)], uv[:, :, a0:a1], r=["uT"], w=[uk])
                for cc in range(DC):
                    P.op("dve", lambda e, bi=bi, cc=cc, tw=tw: e.tensor_scalar(
                        out=vv[:, cc, :tw], in0=uw[bi][:, cc, 0:tw], scalar1=dw[:, cc, 0:1], scalar2=dwb[:, cc:cc + 1],
                        op0=ALU.mult, op1=ALU.add), r=[uk, "dw", "dwb"], w=["vv%d" % cc])
                    for k in range(1, KW):
                        P.op("dve", lambda e, bi=bi, cc=cc, tw=tw, k=k: e.scalar_tensor_tensor(
                            out=vv[:, cc, :tw], in0=uw[bi][:, cc, k:k + tw], scalar=dw[:, cc, k:k + 1], in1=vv[:, cc, :tw],
                            op0=ALU.mult, op1=ALU.add), r=[uk, "dw"], w=["vv%d" % cc])
                    si = self.rot("sqt", 2)
                    P.op("act", lambda e, si=si, cc=cc, tw=tw: e.activation(out=sqt[si][:, :tw], in_=vv[:, cc, :tw],
                                                                          func=AF.Square), r=["vv%d" % cc], w=["sqt%d" % si])
                    P.op("pe", lambda e, cc=cc, tw=tw: e.matmul(pm[:, :tw], self.ones_f[:, :], vv[:, cc, :tw],
                                                              start=(cc == 0), stop=(cc == DC - 1)),
                         r=["vv%d" % cc, "ones_f"], w=["pmn"])
                    P.op("pe", lambda e, cc=cc, tw=tw, si=si: e.matmul(p2[:, :tw], self.ones_f[:, :], sqt[si][:, :tw],
                                                                     start=(cc == 0), stop=(cc == DC - 1)),
                         r=["sqt%d" % si, "ones_f"], w=["pm2"])
                P.op("dve", lambda e, tw=tw: e.tensor_scalar(out=mu[:, :tw], in0=pm[:, :tw], scalar1=1.0 / D, scalar2=None,
                                                            op0=ALU.mult), r=["pmn"], w=["mu"])
                P.op("dve", lambda e, tw=tw: e.tensor_tensor(out=msq[:, :tw], in0=mu[:, :tw], in1=mu[:, :tw], op=ALU.mult),
                     r=["mu"], w=["msq"])
                P.op("dve", lambda e, tw=tw: e.scalar_tensor_tensor(
                    out=msq[:, :tw], in0=p2[:, :tw], scalar=1.0 / D, in1=msq[:, :tw], op0=ALU.mult, op1=ALU.subtract),
                    r=["pm2", "msq"], w=["msq"])
                self.rsqrt(rstd[:, :tw], msq[:, :tw], 1.0, r=["msq"], w=["rstd"])
                for cc in range(DC):
                    ti = self.rot("t1", 2)
                    P.op("dve", lambda e, ti=ti, cc=cc, tw=tw: e.tensor_tensor(
                        out=t1[ti][:, :tw], in0=vv[:, cc, :tw], in1=mu[:, :tw], op=ALU.subtract),
                        r=["vv%d" % cc, "mu"], w=["t1%d" % ti])
                    P.op("dve", lambda e, ti=ti, tw=tw: e.tensor_tensor(
                        out=t1[ti][:, :tw], in0=t1[ti][:, :tw], in1=rstd[:, :tw], op=ALU.mult),
                        r=["t1%d" % ti, "rstd"], w=["t1%d" % ti])
                    P.op("act", lambda e, ti=ti, cc=cc, bi=bi, tw=tw: e.activation(
                        out=hb[bi][:, cc, :tw], in_=t1[ti][:, :tw], func=AF.Silu, scale=lg[:, cc:cc + 1],
                        bias=lb[:, cc:cc + 1]), r=["t1%d" % ti, "lg", "lb"], w=[hk + "_%d" % cc])
                P.dma("act", nv[:, :, t0:t0 + tw], hb[bi][:, :, :tw], r=[hk + "_%d" % cc for cc in range(DC)], w=["nT"])

    def conv_mixer(self, l):
        self.phase_norm(l, 0)
        self.phase_conv1(l)
        self.phase_conv2(l)
        self.phase_linear_res(l, "conv_pw2_w", "pw2_bT")

    def rms_chunks(self, srcf, dst, nchunk, dim, tw, gcol, pst, rs, sq, pfx):
        P = self.P
        P.op("act", lambda e: e.activation(out=sq[:, :nchunk, :tw], in_=srcf[:, :nchunk, :tw], func=AF.Square),
             r=[pfx + "f%d" % q for q in range(nchunk)], w=[pfx + "sq"])
        for q in range(nchunk):
            P.op("pe", lambda e, q=q: e.matmul(pst[:, :tw], self.ones_f[:, :], sq[:, q, :tw], start=(q == 0),
                                               stop=(q == nchunk - 1)), r=[pfx + "sq", "ones_f"], w=[pfx + "pst"])
        self.rsqrt(rs[:, :tw], pst[:, :tw], 1.0 / dim, r=[pfx + "pst"], w=[pfx + "rs"])
        for q in range(nchunk):
            P.op("dve", lambda e, q=q: e.tensor_tensor(out=sq[:, q, :tw], in0=srcf[:, q, :tw], in1=rs[:, :tw], op=ALU.mult),
                 r=[pfx + "f%d" % q, pfx + "rs", pfx + "sq"], w=[pfx + "sq%d" % q])
            P.op("act", lambda e, q=q: e.activation(out=dst[:, q, :tw], in_=sq[:, q, :tw], func=AF.Identity,
                                                    scale=gcol(q), bias=self.zero_c[:, 0:1]),
                 r=[pfx + "sq%d" % q], w=[pfx + "n%d" % q])

    def phase_mla_q(self, l, h0, H):
        c, P = self.cfg, self.P
        DC, QC, QR, NT = c.DC, c.QC, c.QR, c.NT
        with self.phase():
            Wdq = self.sb("Wdq", [128, DC, QR], BF16)
            Wuq = self.sb("Wuq", [128, QC, H * 192], BF16)
            Wsw = self.sb("Wsw", [128, QC, H, 64], BF16)
            qn_ = self.sb("qnrm", [128, QC])
            nin = self.sbn("nin", 2, [128, DC, 512], BF16)
            cqf = self.sb("cqf", [128, QC, 512])
            sq = self.sb("cqsq", [128, QC, 512])
            rs = self.sb("cqrs", [128, 512])
            cqn = self.sb("cqn", [128, QC, 512], BF16)
            rt = self.sbn("rt", 2, [64, 2, 512])
            ta = self.sbn("rta", 2, [64, 512])
            tb = self.sbn("rtb", 2, [64, 512])
            qna = self.sbn("qna", 2, [128, H, 512], BF16)
            qra = self.sbn("qra", 2, [64, H, 512], BF16)
            pc = self.psn("pcq", 2, [128, 512])
            pq = self.psn("pq", 2, [128, 512])
            p1 = self.psn("pr1", 2, [128, 512])
            p2 = self.psn("pr2", 2, [128, 512])
            I = self.inp
            P.dma("sp", qn_[:], I["q_normT"].ap(), w=["qnrm"])
            wdq = I["mla_w_dq"].ap().rearrange("(c p) o -> p c o", p=128)
            for c0 in range(0, DC, 4):
                P.dma("pool", Wdq[:, c0:c0 + 4, :], wdq[:, c0:c0 + 4, :], w=["Wdq"])
            wuq = I["mla_w_uq"].ap().rearrange("(c p) o -> p c o", p=128)
            wuq4 = I["mla_w_uq"].ap().rearrange("(c p) (h x) -> p c h x", p=128, x=192)
            for qc in range(QC):
                P.dma("pool", Wuq[:, qc, :], wuq[:, qc, h0 * 192:(h0 + H) * 192], w=["Wuq"])
                P.dma("pool", Wsw[:, qc, :, 0:32], wuq4[:, qc, h0:h0 + H, 160:192], w=["Wsw"])
                P.dma("pool", Wsw[:, qc, :, 32:64], wuq4[:, qc, h0:h0 + H, 128:160], w=["Wsw"])
            nv = self.scr["nT"].ap().rearrange("(c p) n -> p c n", p=128)
            ropev = I["rope_mla"].ap().rearrange("a p n -> p a n")
            for (t0, tw, v) in c.tiles():
                bi = self.rot("nin", 2)
                nk, rk, qk, qrk = "nin%d" % bi, "rt%d" % bi, "qna%d" % bi, "qra%d" % bi
                P.dma("sp", nin[bi][:, :, :tw], nv[:, :, t0:t0 + tw], r=["nT"], w=[nk])
                P.dma("sp", rt[bi][:, :, :tw], ropev[:, :, t0:t0 + tw], w=[rk])
                for qc in range(QC):
                    pi = self.rot("pcq", 2)
                    for cc in range(DC):
                        P.op("pe", lambda e, pi=pi, qc=qc, cc=cc, bi=bi, tw=tw: e.matmul(
                            pc[pi][:, :tw], Wdq[:, cc, qc * 128:(qc + 1) * 128], nin[bi][:, cc, :tw],
                            start=(cc == 0), stop=(cc == DC - 1)), r=["Wdq", nk], w=["pcq%d" % pi])
                    P.op("act", lambda e, pi=pi, qc=qc, tw=tw: e.activation(out=cqf[:, qc, :tw], in_=pc[pi][:, :tw],
                                                                          func=AF.Copy), r=["pcq%d" % pi], w=["cqf%d" % qc])
                pst = pc[self.rot("pcq", 2)]
                self.rms_chunks(cqf, cqn, QC, QR, tw, lambda q: qn_[:, q:q + 1], pst, rs, sq, "cq")
                cqk = ["cqn%d" % q for q in range(QC)]
                for h in range(H):
                    pi = self.rot("pq", 2)
                    for qc in range(QC):
                        P.op("pe", lambda e, pi=pi, qc=qc, h=h, tw=tw: e.matmul(
                            pq[pi][:, :tw], Wuq[:, qc, h * 192:h * 192 + 128], cqn[:, qc, :tw],
                            start=(qc == 0), stop=(qc == QC - 1)), r=["Wuq"] + cqk, w=["pq%d" % pi])
                    for qc in range(QC):
                        P.op("pe", lambda e, pi=pi, qc=qc, h=h, tw=tw: e.matmul(
                            p1[pi][0:64, :tw], Wuq[:, qc, h * 192 + 128:h * 192 + 192], cqn[:, qc, :tw],
                            start=(qc == 0), stop=(qc == QC - 1)), r=["Wuq"] + cqk, w=["pr1%d" % pi])
                    for qc in range(QC):
                        P.op("pe", lambda e, pi=pi, qc=qc, h=h, tw=tw: e.matmul(
                            p2[pi][0:64, :tw], Wsw[:, qc, h, :], cqn[:, qc, :tw],
                            start=(qc == 0), stop=(qc == QC - 1)), r=["Wsw"] + cqk, w=["pr2%d" % pi])
                    P.op("act", lambda e, pi=pi, h=h, bi=bi, tw=tw: e.activation(out=qna[bi][:, h, :tw], in_=pq[pi][:, :tw],
                                                                                func=AF.Copy), r=["pq%d" % pi], w=[qk + "_%d" % h])
                    P.op("dve", lambda e, pi=pi, bi=bi, tw=tw: e.tensor_tensor(
                        out=ta[pi][:, :tw], in0=p1[pi][0:64, :tw], in1=rt[bi][:, 0, :tw], op=ALU.mult),
                        r=["pr1%d" % pi, rk], w=["rta%d" % pi])
                    P.op("dve", lambda e, pi=pi, bi=bi, tw=tw: e.tensor_tensor(
                        out=tb[pi][:, :tw], in0=p2[pi][0:64, :tw], in1=rt[bi][:, 1, :tw], op=ALU.mult),
                        r=["pr2%d" % pi, rk], w=["rtb%d" % pi])
                    P.op("pool", lambda e, pi=pi, bi=bi, h=h, tw=tw: e.tensor_tensor(
                        out=qra[bi][:, h, :tw], in0=ta[pi][:, :tw], in1=tb[pi][:, :tw], op=ALU.add),
                        r=["rta%d" % pi, "rtb%d" % pi], w=[qrk + "_%d" % h])
                P.dma("act", self.scr["qnT"].ap()[:, h0:h0 + H, t0:t0 + tw], qna[bi][:, :, :tw],
                      r=[qk + "_%d" % h for h in range(H)], w=["qnT"])
                P.dma("act", self.scr["qrT"].ap()[:, h0:h0 + H, t0:t0 + tw], qra[bi][:, :, :tw],
                      r=[qrk + "_%d" % h for h in range(H)], w=["qrT"])

    def phase_mla_kv(self, l):
        c, P = self.cfg, self.P
        DC, KVC, KVR, H, NT = c.DC, c.KVC, c.KVR, c.H, c.NT
        HV = H * 128
        with self.phase():
            Wd = self.sb("Wdkv", [128, DC, KVR + 64], BF16)
            Wks = self.sb("Wkrs", [128, DC, 64], BF16)
            Wk = self.sb("Wk", [128, KVC, H, 128], BF16)
            Wv = self.sb("Wv", [128, KVC, H, 128], BF16)
            kvn_ = self.sb("kvnrm", [128, KVC])
            nin = self.sbn("nin", 2, [128, DC, 512], BF16)
            ckf = self.sb("ckf", [128, KVC, 512])
            sq = self.sb("cksq", [128, KVC, 512])
            rs = self.sb("ckrs", [128, 512])
            ckn = self.sb("ckn", [128, KVC, 512], BF16)
            rt = self.sbn("rt", 2, [64, 2, 512])
            ta = self.sb("rta", [64, 512])
            tb = self.sb("rtb", [64, 512])
            krs = self.sbn("krs", 2, [64, 512], BF16)
            kna = self.sbn("kna", 2, [128, H, 512], BF16)
            vt = self.sbn("vt", 2, [128, HV], BF16)
            pk = self.psn("pkv", 2, [128, 512])
            p1 = self.ps("pr1", [128, 512])
            p2 = self.ps("pr2", [128, 512])
            pn = self.psn("pkn", 2, [128, 512])
            pv = self.psn("pv", 2, [128, 512])
            I = self.inp
            P.dma("sp", kvn_[:], I["kv_normT"].ap(), w=["kvnrm"])
            wd = I["mla_w_dkv"].ap().rearrange("(c p) o -> p c o", p=128)
            for c0 in range(0, DC, 4):
                P.dma("pool", Wd[:, c0:c0 + 4, :], wd[:, c0:c0 + 4, :], w=["Wdkv"])
            P.dma("pool", Wks[:, :, 0:32], wd[:, :, KVR + 32:KVR + 64], w=["Wkrs"])
            P.dma("pool", Wks[:, :, 32:64], wd[:, :, KVR:KVR + 32], w=["Wkrs"])
            wu4 = I["mla_w_ukv"].ap().rearrange("(c p) (h x) -> p c h x", p=128, x=256)
            for kc in range(KVC):
                P.dma("pool", Wk[:, kc, :, :], wu4[:, kc, :, 0:128], w=["Wk"])
                P.dma("pool", Wv[:, kc, :, :], wu4[:, kc, :, 128:256], w=["Wv"])
            nv = self.scr["nT"].ap().rearrange("(c p) n -> p c n", p=128)
            ropev = I["rope_mla"].ap().rearrange("a p n -> p a n")
            for (t0, tw, v) in c.tiles():
                bi = self.rot("nin", 2)
                nk, rk, kk, krk = "nin%d" % bi, "rt%d" % bi, "kna%d" % bi, "krs%d" % bi
                P.dma("sp", nin[bi][:, :, :tw], nv[:, :, t0:t0 + tw], r=["nT"], w=[nk])
                P.dma("sp", rt[bi][:, :, :tw], ropev[:, :, t0:t0 + tw], w=[rk])
                for kc in range(KVC):
                    pi = self.rot("pkv", 2)
                    for cc in range(DC):
                        P.op("pe", lambda e, pi=pi, kc=kc, cc=cc, bi=bi, tw=tw: e.matmul(
                            pk[pi][:, :tw], Wd[:, cc, kc * 128:(kc + 1) * 128], nin[bi][:, cc, :tw],
                            start=(cc == 0), stop=(cc == DC - 1)), r=["Wdkv", nk], w=["pkv%d" % pi])
                    P.op("act", lambda e, pi=pi, kc=kc, tw=tw: e.activation(out=ckf[:, kc, :tw], in_=pk[pi][:, :tw],
                                                                          func=AF.Copy), r=["pkv%d" % pi], w=["ckf%d" % kc])
                for cc in range(DC):
                    P.op("pe", lambda e, cc=cc, bi=bi, tw=tw: e.matmul(
                        p1[0:64, :tw], Wd[:, cc, KVR:KVR + 64], nin[bi][:, cc, :tw], start=(cc == 0), stop=(cc == DC - 1)),
                        r=["Wdkv", nk], w=["pr1"])
                for cc in range(DC):
                    P.op("pe", lambda e, cc=cc, bi=bi, tw=tw: e.matmul(
                        p2[0:64, :tw], Wks[:, cc, :], nin[bi][:, cc, :tw], start=(cc == 0), stop=(cc == DC - 1)),
                        r=["Wkrs", nk], w=["pr2"])
                P.op("dve", lambda e, bi=bi, tw=tw: e.tensor_tensor(out=ta[:, :tw], in0=p1[0:64, :tw], in1=rt[bi][:, 0, :tw],
                                                                   op=ALU.mult), r=["pr1", rk], w=["rta"])
                P.op("dve", lambda e, bi=bi, tw=tw: e.tensor_tensor(out=tb[:, :tw], in0=p2[0:64, :tw], in1=rt[bi][:, 1, :tw],
                                                                   op=ALU.mult), r=["pr2", rk], w=["rtb"])
                P.op("pool", lambda e, bi=bi, tw=tw: e.tensor_tensor(out=krs[bi][:, :tw], in0=ta[:, :tw], in1=tb[:, :tw],
                                                                    op=ALU.add), r=["rta", "rtb"], w=[krk])
                P.dma("act", self.scr["krT"].ap()[:, t0:t0 + tw], krs[bi][:, :tw], r=[krk], w=["krT"])
                pst = pk[self.rot("pkv", 2)]
                self.rms_chunks(ckf, ckn, KVC, KVR, tw, lambda q: kvn_[:, q:q + 1], pst, rs, sq, "ck")
                ckk = ["ckn%d" % q for q in range(KVC)]
                for h in range(H):
                    pi = self.rot("pkn", 2)
                    for kc in range(KVC):
                        P.op("pe", lambda e, pi=pi, kc=kc, h=h, tw=tw: e.matmul(
                            pn[pi][:, :tw], Wk[:, kc, h, :], ckn[:, kc, :tw], start=(kc == 0), stop=(kc == KVC - 1)),
                            r=["Wk"] + ckk, w=["pkn%d" % pi])
                    if h % 2 == 0:
                        P.op("act", lambda e, pi=pi, h=h, bi=bi, tw=tw: e.activation(
                            out=kna[bi][:, h, :tw], in_=pn[pi][:, :tw], func=AF.Copy), r=["pkn%d" % pi], w=[kk + "_%d" % h])
                    else:
                        P.op("dve", lambda e, pi=pi, h=h, bi=bi, tw=tw: e.tensor_copy(
                            out=kna[bi][:, h, :tw], in_=pn[pi][:, :tw]), r=["pkn%d" % pi], w=[kk + "_%d" % h])
                P.dma("act", self.scr["knT"].ap()[:, 0:H, t0:t0 + tw], kna[bi][:, :, :tw],
                      r=[kk + "_%d" % h for h in range(H)], w=["knT"])
                for ts in range(tw // 128):
                    vi = self.rot("vt", 2)
                    vk = "vt%d" % vi
                    for hg in range(HV // 512):
                        pi = self.rot("pv", 2)
                        for kc in range(KVC):
                            P.op("pe", lambda e, pi=pi, kc=kc, hg=hg, ts=ts: e.matmul(
                                pv[pi][:, :], ckn[:, kc, ts * 128:(ts + 1) * 128],
                                Wv[:, kc, hg * 4:(hg + 1) * 4, :].rearrange("p h d -> p (h d)"),
                                start=(kc == 0), stop=(kc == KVC - 1)), r=["Wv"] + ckk, w=["pv%d" % pi])
                        P.op("dve", lambda e, pi=pi, vi=vi, hg=hg: e.tensor_copy(
                            out=vt[vi][:, hg * 512:(hg + 1) * 512], in_=pv[pi][:, :]), r=["pv%d" % pi], w=[vk + "_%d" % hg])
                    r0 = t0 + ts * 128
                    P.dma("act", self.scr["vtok"].ap()[r0:r0 + 128, :], vt[vi][:, :],
                          r=[vk + "_%d" % hg for hg in range(HV // 512)], w=["vtok"])

    def phase_mla_attn(self, l):
        c, P = self.cfg, self.P
        H, NT, S, CTX = c.H, c.NT, c.S, c.CTX
        NKT = NT // 128
        scale = 192.0 ** -0.5
        with self.phase():
            kn = self.sb("kn", [128, NT], BF16)
            kr = self.sb("kr", [64, NT], BF16)
            vh = self.sb("vh", [128, NKT, 128], BF16)
            qn = self.sbn("qn", 2, [128, 512], BF16)
            qr = self.sbn("qr", 2, [64, 512], BF16)
            pt = self.sbn("pT", 3, [128, 512], BF16)
            rd = self.sb("rd", [128, 512])
            dacc = self.sbn("dacc", 2, [128, 512])
            ob = self.sbn("ob", 2, [128, 512], BF16)
            pS = self.psn("pS", 2, [128, 512])
            pO = self.psn("pO", 2, [128, 512])
            pD = self.psn("pD", 2, [128, 512])
            P.dma("sp", kr[:], self.scr["krT"].ap(), r=["krT"], w=["kr"])
            for h in range(H):
                P.dma("sp", kn[:], self.scr["knT"].ap()[:, h, :], r=["knT"], w=["kn"])
                vsrc = self.scr["vtok"].ap()[:, h * 128:(h + 1) * 128].rearrange("(t p) d -> p t d", p=128)
                for k0_ in range(0, NKT, 8):
                    k1_ = min(NKT, k0_ + 8)
                    P.dma("sp", vh[:, k0_:k1_, :], vsrc[:, k0_:k1_, :], r=["vtok"], w=["vh"])
                for (t0, tw, v) in c.tiles(ctx=(l < c.L - 1)):
                    qi = self.rot("qn", 2)
                    qk = "q%d" % qi
                    P.dma("sp", qn[qi][:, :tw], self.scr["qnT"].ap()[:, h, t0:t0 + tw], r=["qnT"], w=[qk + "n"])
                    P.dma("sp", qr[qi][:, :tw], self.scr["qrT"].ap()[:, h, t0:t0 + tw], r=["qrT"], w=[qk + "r"])
                    kts = list(range(NKT)) if v == 0 else list(range(S // 128, NKT))
                    oi = self.rot("pO", 2)
                    for n_, kt in enumerate(kts):
                        si = self.rot("pS", 2)
                        P.op("pe", lambda e, si=si, kt=kt, qi=qi, tw=tw: e.matmul(
                            pS[si][:, :tw], kn[:, kt * 128:(kt + 1) * 128], qn[qi][:, :tw], start=True, stop=False),
                            r=["kn", qk + "n"], w=["pS%d" % si])
                        P.op("pe", lambda e, si=si, kt=kt, qi=qi, tw=tw: e.matmul(
                            pS[si][:, :tw], kr[:, kt * 128:(kt + 1) * 128], qr[qi][:, :tw], start=False, stop=True),
                            r=["kr", qk + "r"], w=["pS%d" % si])
                        pi = self.rot("pT", 3)
                        P.op("act", lambda e, si=si, pi=pi, tw=tw: e.activation(
                            out=pt[pi][:, :tw], in_=pS[si][:, :tw], func=AF.Exp, scale=scale), r=["pS%d" % si], w=["pT%d" % pi])
                        P.op("pe", lambda e, oi=oi, pi=pi, kt=kt, tw=tw, n_=n_, nk=len(kts): e.matmul(
                            pO[oi][:, :tw], vh[:, kt, :], pt[pi][:, :tw], start=(n_ == 0), stop=(n_ == nk - 1)),
                            r=["vh", "pT%d" % pi], w=["pO%d" % oi])
                        if n_ == 0:
                            P.op("dve", lambda e, oi=oi, pi=pi, tw=tw: e.tensor_copy(out=dacc[oi][:, :tw], in_=pt[pi][:, :tw]),
                                 r=["pT%d" % pi], w=["dacc%d" % oi])
                        else:
                            P.op("dve", lambda e, oi=oi, pi=pi, tw=tw: e.tensor_tensor(
                                out=dacc[oi][:, :tw], in0=dacc[oi][:, :tw], in1=pt[pi][:, :tw], op=ALU.add),
                                r=["pT%d" % pi, "dacc%d" % oi], w=["dacc%d" % oi])
                    P.op("pe", lambda e, oi=oi, tw=tw: e.matmul(pD[oi][:, :tw], self.ones_f[:, :], dacc[oi][:, :tw],
                                                               start=True, stop=True), r=["ones_f", "dacc%d" % oi], w=["pD%d" % oi])
                    bi = self.rot("ob", 2)
                    P.op("dve", lambda e, oi=oi, tw=tw: e.reciprocal(out=rd[:, :tw], in_=pD[oi][:, :tw]), r=["pD%d" % oi], w=["rd"])
                    P.op("dve", lambda e, oi=oi, bi=bi, tw=tw: e.tensor_tensor(
                        out=ob[bi][:, :tw], in0=pO[oi][:, :tw], in1=rd[:, :tw], op=ALU.mult), r=["pO%d" % oi, "rd"], w=["ob%d" % bi])
                    P.dma("act", self.scr["nT"].ap()[h * 128:(h + 1) * 128, t0:t0 + tw], ob[bi][:, :tw], r=["ob%d" % bi], w=["nT"])

    def mla_mixer(self, l):
        self.phase_norm(l, 0)
        hh = max(1, self.cfg.H // 2)
        for h0 in range(0, self.cfg.H, hh):
            self.phase_mla_q(l, h0, hh)
        self.phase_mla_kv(l)
        self.phase_mla_attn(l)
        self.phase_linear_res(l, "mla_w_o", None, ctx=(l < self.cfg.L - 1))

    def phase_diff_proj(self, col0, dst, blk0, nblk):
        c, P = self.cfg, self.P
        DC, NT = c.DC, c.NT
        with self.phase():
            W = self.sb("Wp", [128, DC, nblk, 128], BF16)
            Ws = self.sb("Wps", [128, DC, nblk, 128], BF16)
            nin = self.sbn("nin", 2, [128, DC, 512], BF16)
            rt = self.sbn("rt", 2, [128, 2, 512])
            ta = self.sbn("rta", 2, [128, 512])
            tb = self.sbn("rtb", 2, [128, 512])
            qa = self.sbn("qa", 2, [128, nblk, 512], BF16)
            p1 = self.psn("pr1", 2, [128, 512])
            p2 = self.psn("pr2", 2, [128, 512])
            wv = self.inp["diff_w_qkv"].ap().rearrange("(c p) o -> p c o", p=128)
            for b in range(nblk):
                cb = col0 + b * 128
                P.dma("pool", W[:, :, b, :], wv[:, :, cb:cb + 128], w=["Wp"])
                P.dma("pool", Ws[:, :, b, 0:64], wv[:, :, cb + 64:cb + 128], w=["Wps"])
                P.dma("pool", Ws[:, :, b, 64:128], wv[:, :, cb:cb + 64], w=["Wps"])
            nv = self.scr["nT"].ap().rearrange("(c p) n -> p c n", p=128)
            ropev = self.inp["rope_diff"].ap().rearrange("a p n -> p a n")
            for (t0, tw, v) in c.tiles():
                bi = self.rot("nin", 2)
                nk, rk, qk = "nin%d" % bi, "rt%d" % bi, "qa%d" % bi
                P.dma("sp", nin[bi][:, :, :tw], nv[:, :, t0:t0 + tw], r=["nT"], w=[nk])
                P.dma("sp", rt[bi][:, :, :tw], ropev[:, :, t0:t0 + tw], w=[rk])
                for b in range(nblk):
                    pi = self.rot("pr1", 2)
                    for cc in range(DC):
                        P.op("pe", lambda e, pi=pi, b=b, cc=cc, bi=bi, tw=tw: e.matmul(
                            p1[pi][:, :tw], W[:, cc, b, :], nin[bi][:, cc, :tw], start=(cc == 0), stop=(cc == DC - 1)),
                            r=["Wp", nk], w=["pr1%d" % pi])
                    for cc in range(DC):
                        P.op("pe", lambda e, pi=pi, b=b, cc=cc, bi=bi, tw=tw: e.matmul(
                            p2[pi][:, :tw], Ws[:, cc, b, :], nin[bi][:, cc, :tw], start=(cc == 0), stop=(cc == DC - 1)),
                            r=["Wps", nk], w=["pr2%d" % pi])
                    P.op("dve", lambda e, pi=pi, bi=bi, tw=tw: e.tensor_tensor(
                        out=ta[pi][:, :tw], in0=p1[pi][:, :tw], in1=rt[bi][:, 0, :tw], op=ALU.mult),
                        r=["pr1%d" % pi, rk], w=["rta%d" % pi])
                    P.op("dve", lambda e, pi=pi, bi=bi, tw=tw: e.tensor_tensor(
                        out=tb[pi][:, :tw], in0=p2[pi][:, :tw], in1=rt[bi][:, 1, :tw], op=ALU.mult),
                        r=["pr2%d" % pi, rk], w=["rtb%d" % pi])
                    P.op("pool", lambda e, pi=pi, bi=bi, b=b, tw=tw: e.tensor_tensor(
                        out=qa[bi][:, b, :tw], in0=ta[pi][:, :tw], in1=tb[pi][:, :tw], op=ALU.add),
                        r=["rta%d" % pi, "rtb%d" % pi], w=[qk + "_%d" % b])
                P.dma("act", self.scr[dst].ap()[:, blk0:blk0 + nblk, t0:t0 + tw], qa[bi][:, :, :tw],
                      r=[qk + "_%d" % b for b in range(nblk)], w=[dst])

    def phase_diff_v(self):
        c, P = self.cfg, self.P
        DC, D = c.DC, c.D
        with self.phase():
            Wv = self.sb("Wdv", [128, DC, D], BF16)
            nin = self.sbn("nin", 2, [128, DC, 512], BF16)
            vt = self.sbn("vt", 2, [128, D], BF16)
            pv = self.psn("pv", 4, [128, 512])
            wv = self.inp["diff_w_qkv"].ap().rearrange("(c p) o -> p c o", p=128)
            for c0 in range(0, DC, 4):
                P.dma("pool", Wv[:, c0:c0 + 4, :], wv[:, c0:c0 + 4, 2 * D:3 * D], w=["Wdv"])
            nv = self.scr["nT"].ap().rearrange("(c p) n -> p c n", p=128)
            for (t0, tw, v) in c.tiles():
                bi = self.rot("nin", 2)
                nk = "nin%d" % bi
                P.dma("sp", nin[bi][:, :, :tw], nv[:, :, t0:t0 + tw], r=["nT"], w=[nk])
                for ts in range(tw // 128):
                    vi = self.rot("vt", 2)
                    vk = "vt%d" % vi
                    for j in range(D // 512):
                        pi = self.rot("pv", 4)
                        for cc in range(DC):
                            P.op("pe", lambda e, pi=pi, cc=cc, j=j, ts=ts, bi=bi: e.matmul(
                                pv[pi][:, :], nin[bi][:, cc, ts * 128:(ts + 1) * 128], Wv[:, cc, j * 512:(j + 1) * 512],
                                start=(cc == 0), stop=(cc == DC - 1)), r=["Wdv", nk], w=["pv%d" % pi])
                        if j % 2 == 0:
                            P.op("dve", lambda e, pi=pi, vi=vi, j=j: e.tensor_copy(
                                out=vt[vi][:, j * 512:(j + 1) * 512], in_=pv[pi][:, :]), r=["pv%d" % pi], w=[vk + "_%d" % j])
                        else:
                            P.op("act", lambda e, pi=pi, vi=vi, j=j: e.activation(
                                out=vt[vi][:, j * 512:(j + 1) * 512], in_=pv[pi][:, :], func=AF.Copy),
                                r=["pv%d" % pi], w=[vk + "_%d" % j])
                    r0 = t0 + ts * 128
                    P.dma("act", self.scr["vtok"].ap()[r0:r0 + 128, :], vt[vi][:, :],
                          r=[vk + "_%d" % j for j in range(D // 512)], w=["vtok"])

    def phase_diff_attn(self, l):
        c, P = self.cfg, self.P
        HD, NT, S = c.HD, c.NT, c.S
        NKT = NT // 128
        scale = 128.0 ** -0.5
        lam_init = 0.8 - 0.6 * math.exp(-0.3 * l)
        with self.phase():
            lv = self.sb("lv", [128, 4, 128])
            pr = self.sb("lpr", [128, 2, 128])
            s12 = self.sb("s12", [128, 2])
            nlam = self.sb("nlam", [128, 1])
            sg = self.sb("sg", [128, 2])
            k0 = self.sb("k0", [128, NT], BF16)
            k1 = self.sb("k1", [128, NT], BF16)
            vh = self.sb("vh", [128, NKT, 256], BF16)
            q0 = self.sbn("q0", 2, [128, 512], BF16)
            q1 = self.sbn("q1", 2, [128, 512], BF16)
            pt = self.sbn("pT", 4, [128, 512], BF16)
            rd = self.sb("rd", [128, 2, 512])
            dacc = self.sbn("dacc", 2, [128, 512])
            tt = self.sbn("tt", 2, [128, 512])
            aa = self.sb("aa", [128, 2, 512])
            sqa = self.sb("sqa", [128, 2, 512])
            rs = self.sb("rsd", [128, 512])
            ob = self.sbn("ob", 2, [128, 2, 512], BF16)
            pS = self.psn("pS", 2, [128, 512])
            pO = [self.psn("pO%d" % n, 2, [128, 512]) for n in range(2)]
            pD = self.psn("pD", 2, [128, 512])
            lamt = self.inp["diff_lam"]
            P.dma("sp", lv[:], bass.AP(lamt, 0, [[0, 128], [128, 4], [1, 128]]), w=["lv"])
            P.dma("sp", sg[:], self.inp["subln_gT"].ap(), w=["sg"])
            P.op("dve", lambda e: e.tensor_scalar(out=sg[:], in0=sg[:], scalar1=float(1.0 - lam_init), scalar2=None,
                                                  op0=ALU.mult), r=["sg"], w=["sg"])
            for i in range(2):
                P.op("dve", lambda e, i=i: e.tensor_tensor(out=pr[:, i, :], in0=lv[:, 2 * i, :], in1=lv[:, 2 * i + 1, :],
                                                           op=ALU.mult), r=["lv"], w=["lpr"])
                P.op("dve", lambda e, i=i: e.reduce_sum(out=s12[:, i:i + 1], in_=pr[:, i, :], axis=mybir.AxisListType.X),
                     r=["lpr"], w=["s12"])
            P.op("act", lambda e: e.activation(out=s12[:], in_=s12[:], func=AF.Exp), r=["s12"], w=["s12"])
            P.op("dve", lambda e: e.tensor_tensor(out=nlam[:], in0=s12[:, 1:2], in1=s12[:, 0:1], op=ALU.subtract),
                 r=["s12"], w=["nlam"])
            P.op("dve", lambda e: e.tensor_scalar(out=nlam[:], in0=nlam[:], scalar1=float(-lam_init), scalar2=None,
                                                  op0=ALU.add), r=["nlam"], w=["nlam"])
            ks = [k0, k1]
            for h in range(HD):
                for n in range(2):
                    P.dma("sp", ks[n][:], self.scr["knT"].ap()[:, 2 * h + n, :], r=["knT"], w=["k%d" % n])
                vsrc = self.scr["vtok"].ap()[:, h * 256:(h + 1) * 256].rearrange("(t p) d -> p t d", p=128)
                for k0_ in range(0, NKT, 8):
                    k1_ = min(NKT, k0_ + 8)
                    P.dma("sp", vh[:, k0_:k1_, :], vsrc[:, k0_:k1_, :], r=["vtok"], w=["vh"])
                for (t0, tw, v) in c.tiles(ctx=False):
                    qi = self.rot("q0", 2)
                    qs = [q0[qi], q1[qi]]
                    for n in range(2):
                        P.dma("sp", qs[n][:, :tw], self.scr["qnT"].ap()[:, 2 * h + n, t0:t0 + tw], r=["qnT"], w=["q%d_%d" % (n, qi)])
                    for kt in range(NKT):
                        for n in range(2):
                            P.op("pe", lambda e, n=n, kt=kt, qs=qs, tw=tw: e.matmul(
                                pS[n][:, :tw], ks[n][:, kt * 128:(kt + 1) * 128], qs[n][:, :tw], start=True, stop=True),
                                r=["k%d" % n, "q%d_%d" % (n, qi)], w=["pS%d" % n])
                            pi = self.rot("pT", 4)
                            P.op("act", lambda e, n=n, pi=pi, tw=tw: e.activation(
                                out=pt[pi][:, :tw], in_=pS[n][:, :tw], func=AF.Exp, scale=scale), r=["pS%d" % n], w=["pT%d" % pi])
                            for j in range(2):
                                P.op("pe", lambda e, n=n, j=j, pi=pi, kt=kt, tw=tw: e.matmul(
                                    pO[n][j][:, :tw], vh[:, kt, j * 128:(j + 1) * 128], pt[pi][:, :tw],
                                    start=(kt == 0), stop=(kt == NKT - 1)), r=["vh", "pT%d" % pi], w=["pO%d_%d" % (n, j)])
                            if kt == 0:
                                P.op("dve", lambda e, n=n, pi=pi, tw=tw: e.tensor_copy(out=dacc[n][:, :tw], in_=pt[pi][:, :tw]),
                                     r=["pT%d" % pi], w=["dacc%d" % n])
                            else:
                                P.op("dve", lambda e, n=n, pi=pi, tw=tw: e.tensor_tensor(
                                    out=dacc[n][:, :tw], in0=dacc[n][:, :tw], in1=pt[pi][:, :tw], op=ALU.add),
                                    r=["pT%d" % pi, "dacc%d" % n], w=["dacc%d" % n])
                    for n in range(2):
                        P.op("pe", lambda e, n=n, tw=tw: e.matmul(pD[n][:, :tw], self.ones_f[:, :], dacc[n][:, :tw],
                                                                 start=True, stop=True), r=["ones_f", "dacc%d" % n], w=["pD%d" % n])
                    for n in range(2):
                        P.op("dve", lambda e, n=n, tw=tw: e.reciprocal(out=rd[:, n, :tw], in_=pD[n][:, :tw]),
                             r=["pD%d" % n], w=["rd%d" % n])
                    for j in range(2):
                        P.op("dve", lambda e, j=j, tw=tw: e.tensor_tensor(out=tt[0][:, :tw], in0=pO[0][j][:, :tw], in1=rd[:, 0, :tw],
                                                                         op=ALU.mult), r=["pO0_%d" % j, "rd0"], w=["tt0"])
                        P.op("dve", lambda e, j=j, tw=tw: e.tensor_tensor(out=tt[1][:, :tw], in0=pO[1][j][:, :tw], in1=rd[:, 1, :tw],
                                                                         op=ALU.mult), r=["pO1_%d" % j, "rd1"], w=["tt1"])
                        P.op("dve", lambda e, j=j, tw=tw: e.scalar_tensor_tensor(
                            out=aa[:, j, :tw], in0=tt[1][:, :tw], scalar=nlam[:, 0:1], in1=tt[0][:, :tw], op0=ALU.mult, op1=ALU.add),
                            r=["tt0", "tt1", "nlam"], w=["aa%d" % j])
                        P.op("act", lambda e, j=j, tw=tw: e.activation(out=sqa[:, j, :tw], in_=aa[:, j, :tw], func=AF.Square),
                             r=["aa%d" % j], w=["sqa%d" % j])
                    for j in range(2):
                        P.op("pe", lambda e, j=j, tw=tw: e.matmul(pS[0][:, :tw], self.ones_f[:, :], sqa[:, j, :tw],
                                                                  start=(j == 0), stop=(j == 1)), r=["sqa%d" % j, "ones_f"], w=["pS0"])
                    self.rsqrt(rs[:, :tw], pS[0][:, :tw], 1.0 / 256.0, r=["pS0"], w=["rsd"])
                    bi = self.rot("ob", 2)
                    for j in range(2):
                        P.op("dve", lambda e, j=j, tw=tw: e.tensor_tensor(out=aa[:, j, :tw], in0=aa[:, j, :tw], in1=rs[:, :tw],
                                                                         op=ALU.mult), r=["aa%d" % j, "rsd"], w=["aa%d" % j])
                        P.op("act", lambda e, j=j, bi=bi, tw=tw: e.activation(
                            out=ob[bi][:, j, :tw], in_=aa[:, j, :tw], func=AF.Identity, scale=sg[:, j:j + 1],
                            bias=self.zero_c[:, 0:1]), r=["aa%d" % j, "sg"], w=["ob%d_%d" % (bi, j)])
                    P.dma("act", self.scr["nT"].ap().rearrange("(c p) n -> p c n", p=128)[:, 2 * h:2 * h + 2, t0:t0 + tw],
                          ob[bi][:, :, :tw], r=["ob%d_%d" % (bi, j) for j in range(2)], w=["nT"])

    def diff_mixer(self, l):
        c = self.cfg
        self.phase_norm(l, 0)
        nb = 2 * c.HD
        half = max(1, nb // 2)
        for b0 in range(0, nb, half):
            self.phase_diff_proj(b0 * 128, "qnT", b0, half)
        for b0 in range(0, nb, half):
            self.phase_diff_proj(c.D + b0 * 128, "knT", b0, half)
        self.phase_diff_v()
        self.phase_diff_attn(l)
        self.phase_linear_res(l, "diff_w_o", None, ctx=False)

    def build(self, upto=99):
        self.declare()
        self.phase_consts()
        self.phase_copy_in()
        if upto == 99:
            for l in range(self.cfg.L):
                if l % 4 == 0:
                    self.conv_mixer(l)
                if l % 4 == 1:
                    self.mla_mixer(l)
                if l % 4 == 3:
                    self.diff_mixer(l)
                if l % 4 == 2:
                    self.phase_norm(l, 0)
                    self.phase_pool(l)
                self.moe(l)
        if upto == 1:
            self.phase_norm(0, 0)
        if upto == 2:
            self.moe(0)
        if upto == 8:
            self.diff_mixer(3)
        if upto == 7:
            self.mla_mixer(1)
        if upto == 6:
            self.conv_mixer(0)
        if upto == 5:
            self.phase_norm(2, 0)
            self.phase_pool(2)
        if upto == 4:
            self.phase_norm2_router(0)
            self.phase_tok()
        if upto == 3:
            self.phase_norm2_router(0)
            self.phase_tok()
            self.phase_topk(0)
            with self.phase():
                self.P.dma("sp", self.scr["dbg_idx"].ap(), self.idxT[:], w=["dbg"])
                self.P.dma("sp", self.scr["dbg_g"].ap(), self.gT[:], w=["dbg2"])
        self.phase_norm(0, 0, final=True)
        self.P.es.close()
        return self.nc


def host_inputs(cfg, b, x, c, ctx, c_ctx, ada_w, ada_b, norm_g, final_g, conv_pw1_w, conv_pw1_b, conv_dw_w, conv_dw_b,
                conv_ln_g, conv_ln_b, conv_pw2_w, conv_pw2_b, mla_w_dq, mla_q_norm, mla_w_uq, mla_w_dkv,
                mla_kv_norm, mla_w_ukv, mla_w_o, pool_w, pool_scale, diff_w_qkv, diff_lq1, diff_lk1, diff_lq2,
                diff_lk2, diff_subln_g, diff_w_o, moe_router, moe_w_gate, moe_w_up, moe_w_down):
    f = lambda a: np.ascontiguousarray(a, dtype=np.float32)
    D, NT, S, CTX, DC = cfg.D, cfg.NT, cfg.S, cfg.CTX, cfg.DC

    def fm(vec):
        return f(np.asarray(vec).reshape(-1, 128).T)

    m = {}
    m["xT_in"] = f(np.concatenate([np.asarray(x[b]).T, np.asarray(ctx[b]).T], axis=1))
    m["svec"] = f(np.stack([fm(c[b]), fm(c_ctx)], axis=-1))
    m["ada_w"] = f(ada_w)
    m["ada_bT"] = f(np.stack([fm(ada_b[l]) for l in range(cfg.L)], axis=1))
    m["normgT"] = f(np.stack([np.stack([fm(norm_g[l, k]) for k in range(2)], axis=1) for l in range(cfg.L)], axis=1))
    m["finalgT"] = fm(final_g)
    m["conv_pw1_w"] = f(conv_pw1_w); m["pw1_bT"] = fm(conv_pw1_b)
    m["dw_wT"] = f(np.asarray(conv_dw_w).T.reshape(DC, 128, cfg.CONVW).transpose(1, 0, 2))
    m["dw_bT"] = fm(conv_dw_b); m["ln_gT"] = fm(conv_ln_g); m["ln_bT"] = fm(conv_ln_b)
    m["conv_pw2_w"] = f(conv_pw2_w); m["pw2_bT"] = fm(conv_pw2_b)
    m["mla_w_dq"] = f(mla_w_dq); m["q_normT"] = fm(mla_q_norm); m["mla_w_uq"] = f(mla_w_uq)
    m["mla_w_dkv"] = f(mla_w_dkv); m["kv_normT"] = fm(mla_kv_norm); m["mla_w_ukv"] = f(mla_w_ukv)
    m["mla_w_o"] = f(mla_w_o)
    m["pool_w"] = f(pool_w); m["pool_scaleT"] = fm(pool_scale)
    m["diff_w_qkv"] = f(diff_w_qkv)
    m["diff_lam"] = f(np.concatenate([diff_lq1, diff_lk1, diff_lq2, diff_lk2])[None, :])
    m["subln_gT"] = fm(diff_subln_g); m["diff_w_o"] = f(diff_w_o)
    m["moe_router"] = f(moe_router); m["moe_w_gate"] = f(moe_w_gate); m["moe_w_up"] = f(moe_w_up)
    m["moe_w_down"] = f(moe_w_down)
    m["ident"] = np.eye(128, dtype=np.float32)
    m["padidx"] = f(np.tile((NT + np.arange(128, dtype=np.float32))[None, :], (cfg.E, 1)))
    rows = S // cfg.GRID_W

    def rope_tab(rot):
        q = rot // 4
        inv = (10000.0 ** (-np.arange(q, dtype=np.float32) / q)).astype(np.float32)
        ra = np.arange(rows, dtype=np.float32)[:, None, None] * inv
        ca = np.arange(cfg.GRID_W, dtype=np.float32)[None, :, None] * inv
        ang = np.concatenate([np.broadcast_to(ra, (rows, cfg.GRID_W, q)), np.broadcast_to(ca, (rows, cfg.GRID_W, q))], -1)
        ang = ang.reshape(S, 2 * q).astype(np.float32)
        cs, sn = np.cos(ang).T, np.sin(ang).T
        cosf = np.concatenate([np.concatenate([cs, cs], 0), np.ones((rot, CTX), np.float32)], 1)
        sinf = np.concatenate([np.concatenate([-sn, sn], 0), np.zeros((rot, CTX), np.float32)], 1)
        return f(np.stack([cosf, sinf]))
    m["rope_mla"] = rope_tab(64)
    m["rope_diff"] = rope_tab(128)
    inv = np.zeros((4, NT), np.float32)
    for g, w in enumerate((2, 4, 8, 16)):
        for (off, ln) in ((0, S), (S, CTX)):
            t = np.arange(ln)
            lo = np.clip(t - w // 2, 0, ln); hi = np.clip(t - w // 2 + w, 0, ln)
            inv[g, off:off + ln] = 1.0 / (hi - lo).astype(np.float32)
    m["pool_inv"] = inv
    return m


_CFG = Cfg()


def kernel(**inputs):
    cfg = _CFG
    bld = Builder(cfg)
    nc = bld.build()
    in_maps = [host_inputs(cfg, b, **inputs) for b in range(cfg.B)]
    res = run_bass_kernel_spmd(nc, in_maps, core_ids=list(range(cfg.B)))
    out = np.stack([np.ascontiguousarray(res.results[b]["outT"].T) for b in range(cfg.B)], axis=0)
    return out.astype(np.float32)
```
